# Optimizing a Trainium2 kernel written in Bass

```python
import jax, jax.numpy as jnp
from jax import lax
import numpy as np

D_MODEL = 1024
BATCH = 8
SEQ = 4096
DEPTH = 1

CHUNK = 64
EPS = 1e-6
GLA_HEADS = 4
GLA_DK = 128
GLA_DV = 128
GLA_LOWRANK = 16
GLA_TAU = 16.0
GLA_QK = GLA_HEADS * GLA_DK
GLA_V = GLA_HEADS * GLA_DV
SGU_GROUPS = 4
SGU_GROUP_DIM = 128
SGU_BLOCK = 128
SGU_W = SGU_GROUPS * SGU_GROUP_DIM
PEER_HEADS = 8
PEER_DKEY = 256
PEER_NKEYS = 128
PEER_TOPK = 16
PEER_EXPERTS = PEER_NKEYS * PEER_NKEYS
PEER_TOKEN_BLOCK = 128
IN_COLS = (GLA_QK, GLA_QK, GLA_V, GLA_V, GLA_LOWRANK, SGU_W, SGU_W, D_MODEL, D_MODEL)
IN_WIDTH = sum(IN_COLS)

kernel_name = "hybrid_gla_sgu_peer_block"


def rmsnorm(x, g):
    xf = x.astype(jnp.float32)
    y = xf * lax.rsqrt(jnp.mean(xf * xf, axis=-1, keepdims=True) + EPS)
    return (y * g.astype(jnp.float32)).astype(x.dtype)


def gla_branch(q, k, v, r, a_lr, w_a2, b_a, g_out):
    B, S, _ = q.shape
    NC = S // CHUNK
    f32 = jnp.float32
    qc = q.astype(f32).reshape(B, NC, CHUNK, GLA_HEADS, GLA_DK) * (GLA_DK ** -0.5)
    kc = k.astype(f32).reshape(B, NC, CHUNK, GLA_HEADS, GLA_DK)
    vc = v.astype(f32).reshape(B, NC, CHUNK, GLA_HEADS, GLA_DV)
    log_a = jax.nn.log_sigmoid((a_lr @ w_a2 + b_a).astype(f32)) / GLA_TAU
    log_a = log_a.reshape(B, NC, CHUNK, GLA_HEADS, GLA_DK)
    cum = jnp.cumsum(log_a, axis=2)
    total = cum[:, :, -1]
    k_dec = kc * jnp.exp(total[:, :, None] - cum)
    chunk_kv = jnp.einsum('bnchk,bnchv->bnhkv', k_dec, vc)
    decay = jnp.exp(total)

    def step(state, inp):
        dec, kv, qq = inp
        state = dec[..., None] * state + kv
        out = jnp.einsum('bchk,bhkv->bchv', qq, state)
        return state, out

    init = jnp.zeros((B, GLA_HEADS, GLA_DK, GLA_DV), f32)
    _, o = lax.scan(step, init, (jnp.moveaxis(decay, 1, 0), jnp.moveaxis(chunk_kv, 1, 0),
                                 jnp.moveaxis(qc, 1, 0)))
    o = jnp.moveaxis(o, 0, 1).reshape(B, S, GLA_HEADS, GLA_DV)
    o = o * lax.rsqrt(jnp.mean(o * o, axis=-1, keepdims=True) + EPS)
    o = o.reshape(B, S, GLA_V) * g_out.astype(f32)
    return (o * jax.nn.silu(r.astype(f32))).astype(q.dtype)


def sgu_branch(u, v, ln_g, ln_b, w_s, b_s):
    B, S, _ = v.shape
    f32 = jnp.float32
    vf = v.astype(f32)
    mu = jnp.mean(vf, axis=-1, keepdims=True)
    var = jnp.mean(jnp.square(vf - mu), axis=-1, keepdims=True)
    vn = (vf - mu) * lax.rsqrt(var + EPS) * ln_g.astype(f32) + ln_b.astype(f32)
    NB = S // SGU_BLOCK
    vn = vn.reshape(B, NB, SGU_BLOCK, SGU_GROUPS, SGU_GROUP_DIM)
    pos = jnp.arange(SGU_BLOCK) // CHUNK
    mask = pos[:, None] >= pos[None, :]
    w = jnp.where(mask[None], w_s.astype(f32), 0.0)
    mixed = jnp.einsum('gij,bnjgc->bnigc', w, vn) + b_s.astype(f32).T[None, None, :, :, None]
    return (u.astype(f32) * mixed.reshape(B, S, SGU_W)).astype(u.dtype)


def peer(x, w_q, sub_k1, sub_k2, expert_u, expert_v):
    B, S, D = x.shape
    T = B * S
    f32 = jnp.float32
    xt = x.reshape(T, D)
    q = (xt @ w_q).astype(f32).reshape(T, PEER_HEADS, 2, PEER_DKEY // 2)
    s1 = jnp.einsum('thd,kd->thk', q[:, :, 0], sub_k1.astype(f32))
    s2 = jnp.einsum('thd,kd->thk', q[:, :, 1], sub_k2.astype(f32))
    v1, i1 = lax.top_k(s1, PEER_TOPK)
    v2, i2 = lax.top_k(s2, PEER_TOPK)
    cand = (v1[..., :, None] + v2[..., None, :]).reshape(T, PEER_HEADS, PEER_TOPK * PEER_TOPK)
    vals, ci = lax.top_k(cand, PEER_TOPK)
    ia = ci // PEER_TOPK
    ib = ci % PEER_TOPK
    idx = jnp.take_along_axis(i1, ia, axis=-1) * PEER_NKEYS + jnp.take_along_axis(i2, ib, axis=-1)
    gates = jax.nn.softmax(vals, axis=-1)
    NBLK = T // PEER_TOKEN_BLOCK
    HK = PEER_HEADS * PEER_TOPK
    idx = idx.reshape(NBLK, PEER_TOKEN_BLOCK, HK)
    gates = gates.reshape(NBLK, PEER_TOKEN_BLOCK, HK)
    xb = xt.reshape(NBLK, PEER_TOKEN_BLOCK, D)

    def block(args):
        xx, ii, gg = args
        u = expert_u[ii]
        h = jnp.einsum('tkd,td->tk', u, xx).astype(f32)
        a = (jax.nn.gelu(h) * gg).astype(x.dtype)
        return jnp.einsum('tk,tkd->td', a, expert_v[ii])

    out = lax.map(block, (xb, idx, gates))
    return out.reshape(B, S, D)


def setup_inputs(seed: int = 0) -> dict:
    key = jax.random.key(seed)
    ks = jax.random.split(key, 24)
    f32 = jnp.float32
    L, D = DEPTH, D_MODEL
    nrm = lambda k, shape, scale: jax.random.normal(k, shape, f32) * scale
    return {
        "x": jax.random.normal(ks[0], (BATCH, SEQ, D), f32),
        "norm1_g": 1.0 + nrm(ks[1], (L, D), 0.02),
        "w_in": nrm(ks[2], (L, D, IN_WIDTH), D ** -0.5),
        "w_gate_up": nrm(ks[3], (L, GLA_LOWRANK, GLA_QK), GLA_LOWRANK ** -0.5),
        "b_gate": 1.0 + nrm(ks[4], (L, GLA_QK), 0.1),
        "gla_norm_g": 1.0 + nrm(ks[5], (L, GLA_V), 0.02),
        "sgu_ln_g": 1.0 + nrm(ks[6], (L, SGU_W), 0.02),
        "sgu_ln_b": nrm(ks[7], (L, SGU_W), 0.02),
        "sgu_w": nrm(ks[8], (L, SGU_GROUPS, SGU_BLOCK, SGU_BLOCK), 0.5 * SGU_BLOCK ** -0.5),
        "sgu_b": 1.0 + nrm(ks[9], (L, SGU_GROUPS, SGU_BLOCK), 0.02),
        "w_branch_a": nrm(ks[10], (L, GLA_V, D), GLA_V ** -0.5),
        "w_branch_b": nrm(ks[11], (L, SGU_W, D), SGU_W ** -0.5),
        "w_out": nrm(ks[12], (L, D, D), D ** -0.5),
        "norm2_g": 1.0 + nrm(ks[13], (L, D), 0.02),
        "peer_wq": nrm(ks[14], (L, D, PEER_HEADS * PEER_DKEY), D ** -0.5),
        "peer_k1": nrm(ks[15], (L, PEER_NKEYS, PEER_DKEY // 2), (PEER_DKEY // 2) ** -0.5),
        "peer_k2": nrm(ks[16], (L, PEER_NKEYS, PEER_DKEY // 2), (PEER_DKEY // 2) ** -0.5),
        "peer_u": nrm(ks[17], (L, PEER_EXPERTS, D), D ** -0.5),
        "peer_v": nrm(ks[18], (L, PEER_EXPERTS, D), PEER_HEADS ** -0.5),
        "final_g": 1.0 + nrm(ks[19], (D,), 0.02),
    }


def reference(x, norm1_g, w_in, w_gate_up, b_gate, gla_norm_g, sgu_ln_g, sgu_ln_b, sgu_w, sgu_b,
              w_branch_a, w_branch_b, w_out, norm2_g, peer_wq, peer_k1, peer_k2, peer_u, peer_v,
              final_g):
    offs = np.cumsum(IN_COLS)[:-1].tolist()
    h = x
    for l in range(DEPTH):
        n1 = rmsnorm(h, norm1_g[l])
        proj = n1 @ w_in[l]
        q, k, v, r, a_lr, su, sv, ga, gb = jnp.split(proj, offs, axis=-1)
        y_a = gla_branch(q, k, v, r, a_lr, w_gate_up[l], b_gate[l], gla_norm_g[l])
        su = jax.nn.gelu(su)
        sv = jax.nn.gelu(sv)
        y_b = sgu_branch(su, sv, sgu_ln_g[l], sgu_ln_b[l], sgu_w[l], sgu_b[l])
        merged = (jax.nn.sigmoid(ga) * (y_a @ w_branch_a[l])
                  + jax.nn.sigmoid(gb) * (y_b @ w_branch_b[l]))
        h = h + merged @ w_out[l]
        n2 = rmsnorm(h, norm2_g[l])
        h = h + peer(n2, peer_wq[l], peer_k1[l], peer_k2[l], peer_u[l], peer_v[l])
    return rmsnorm(h, final_g)
```

```python
import numpy as np
from contextlib import ExitStack
from collections import deque
import concourse.bass as bass
import concourse.mybir as mybir
from concourse.bass_utils import run_bass_kernel_spmd

F32 = mybir.dt.float32
BF16 = mybir.dt.bfloat16
I32 = mybir.dt.int32
U32 = mybir.dt.uint32
AF = mybir.ActivationFunctionType
ALU = mybir.AluOpType
AX = mybir.AxisListType

D = 1024
SEQ = 4096
NCORES = 8
EPS = 1e-6
NEXP = 16384
NBLK = 14
NWALL = 18
WIN_COL0 = [0, 512, 1024, 1536, 2064, 2576, 3088, 3600, 4112, 4624]

CA = {}
_o = 0
for _n, _w in [("trirev", 128), ("chunkind", 2), ("iota16", 16), ("g1T", 8), ("g2T", 8), ("goutT", 4),
               ("sbT", 4), ("lng", 512), ("lnb", 512), ("g2bc", 1024), ("gFbc", 1024), ("wga", 512)]:
    CA[_n] = (_o, _o + _w)
    _o += _w
NCA = _o
CB = {}
_o = 0
for _n, _w in [("ident", 128), ("k1T", 128), ("k2T", 128), ("swT", 512), ("mask", 128), ("walr", 128)]:
    CB[_n] = (_o, _o + _w)
    _o += _w
NCB = _o


class Tok:
    __slots__ = ("sem", "key", "val", "eng")

    def __init__(self, sem, key, val, eng):
        self.sem, self.key, self.val, self.eng = sem, key, val, eng


class Eng:
    def __init__(self, name, sem):
        self.name, self.sem = name, sem
        self.n = 0
        self.seen = {}
        self.prog = []


class Sched:
    ENGS = ("pe", "act", "dve", "pool", "sp")

    def __init__(self, nc, stack):
        self.nc = nc
        self.stack = stack
        self.E = {}
        for n in self.ENGS:
            sem = stack.enter_context(nc.semaphore("e_" + n))
            self.E[n] = Eng(n, sem)
        self.dsem = {}
        self.lastw = {}
        self.readers = {}

    def _dsem(self, key):
        if key not in self.dsem:
            sem = self.stack.enter_context(self.nc.semaphore("d_" + key))
            self.dsem[key] = [sem, 0]
        return self.dsem[key]

    def _wait(self, eng, tok):
        if eng.seen.get(tok.key, 0) >= tok.val:
            return
        eng.seen[tok.key] = tok.val
        eng.prog.append(lambda h, s=tok.sem, v=tok.val: h.wait_ge(s, v))

    def op(self, engname, fn, reads=(), writes=()):
        eng = self.E[engname]
        for k in reads:
            w = self.lastw.get(k)
            if w is not None and not (w.eng is eng and engname == "pe"):
                self._wait(eng, w)
        for k in writes:
            w = self.lastw.get(k)
            if w is not None and not (w.eng is eng and engname == "pe"):
                self._wait(eng, w)
            for r in self.readers.get(k, {}).values():
                if not (r.eng is eng and engname == "pe"):
                    self._wait(eng, r)
        eng.n += 1
        tok = Tok(eng.sem, "e_" + engname, eng.n, eng)
        eng.prog.append(lambda h, f=fn, s=eng.sem: f(h).then_inc(s, 1))
        self._record(tok, reads, writes)
        return tok

    def dma(self, engname, fn, semkey, reads=(), writes=()):
        eng = self.E[engname]
        for k in reads:
            w = self.lastw.get(k)
            if w is not None:
                self._wait(eng, w)
        for k in writes:
            w = self.lastw.get(k)
            if w is not None:
                self._wait(eng, w)
            for r in self.readers.get(k, {}).values():
                self._wait(eng, r)
        d = self._dsem(semkey)
        d[1] += 16
        tok = Tok(d[0], "d_" + semkey, d[1], None)
        eng.prog.append(lambda h, f=fn, s=d[0]: f(h).then_inc(s, 16))
        self._record(tok, reads, writes)
        return tok

    def _record(self, tok, reads, writes):
        for k in reads:
            self.readers.setdefault(k, {})[tok.key] = tok
        for k in writes:
            self.lastw[k] = tok
            self.readers[k] = {}

    def emit(self):
        hmap = {"pe": "tensor", "act": "scalar", "dve": "vector", "pool": "gpsimd", "sp": "sync"}
        with self.nc.Block() as block:
            for n in self.ENGS:
                prog = self.E[n].prog

                def body(h, prog=prog):
                    for t in prog:
                        t(h)

                getattr(block, hmap[n])(body)


class Defer:
    def __init__(self):
        self.items = []

    def op(self, *a, **k):
        self.items.append(("op", a, k))

    def dma(self, *a, **k):
        self.items.append(("dma", a, k))


def build_nc(NT=32, taps=None):
    taps = taps or []
    nc = bass.Bass("TRN2", target_bir_lowering=False)
    x_d = nc.dram_tensor("x", [SEQ, D], F32, kind="ExternalInput").ap()
    wall_d = nc.dram_tensor("wall", [NWALL, 128, 4096], F32, kind="ExternalInput").ap()
    cA_d = nc.dram_tensor("cA", [128, NCA], F32, kind="ExternalInput").ap()
    cB_d = nc.dram_tensor("cB", [128, NCB], F32, kind="ExternalInput").ap()
    puv_d = nc.dram_tensor("peer_uv", [NEXP, 2 * D], F32, kind="ExternalInput").ap()
    y_d = nc.dram_tensor("y", [SEQ, D], F32, kind="ExternalOutput").ap()
    wsc_d = nc.dram_tensor("wsc", [NBLK, 128, 4096], BF16, kind="Internal").ap()
    tbl_d = nc.dram_tensor("tbl", [NEXP, 2 * D], BF16, kind="Internal").ap()
    tap_d = {}

    with ExitStack() as st:
        st.enter_context(nc.allow_low_precision("bf16 matmul operands, fp32 accumulation"))
        S = Sched(nc, st)

        sb_total = [0]

        def sb(name, shape, dt):
            n = 1
            for d_ in shape[1:]:
                n *= d_
            sb_total[0] += n * (2 if dt == BF16 else 4)
            return st.enter_context(nc.sbuf_tensor(name, shape, dt))

        def ps(name, shape, dt):
            return st.enter_context(nc.psum_tensor(name, shape, dt))

        cA = sb("cA_sb", [128, NCA], F32)
        ident = sb("ident", [128, 128], BF16)
        k1T = sb("k1T", [128, 128], BF16)
        k2T = sb("k2T", [128, 128], BF16)
        swT = sb("swT", [128, 4, 128], BF16)
        walr = sb("walr", [128, 8, 16], BF16)
        wa = sb("wa", [128, 4, 1024], BF16)
        wb = sb("wb", [128, 4, 1024], BF16)
        wo = sb("wo", [128, 8, 1024], BF16)
        NWB = 3
        wbuf = [sb(f"wbuf{i}", [128, 8, 512], BF16) for i in range(NWB)]
        NR = 13
        ring = sb("ring", [128, NR * 2048], BF16)
        NDG = 8
        dg = sb("dg", [128, NDG, 128], BF16)
        acc = [sb(f"acc{i}", [128, 1024], F32) for i in range(2)]
        state = sb("state", [128, 512], F32)
        state_bf = [sb(f"state_bf{i}", [128, 512], BF16) for i in range(2)]
        junk_a = sb("junk_a", [128, 1024], BF16)
        jd_raw = sb("jd_raw", [128, 512], F32)
        junk_d = jd_raw[:].bitcast(BF16)
        s_wk_t = sb("s_wk", [128, 256], F32)
        s_wk = s_wk_t[:]
        xn = sb("xn", [128, 1024], BF16)
        nT = sb("nT", [128, 8, 128], BF16)
        qT = sb("qT", [128, 512], BF16)
        k_sb = sb("k_sb", [128, 512], F32)
        v_bf = sb("v_bf", [128, 512], BF16)
        silu_r = sb("silu_r", [128, 512], F32)
        sugv = sb("sugv", [128, 1024], F32)
        sga = sb("sga", [128, 1024], F32)
        sgb = sb("sgb", [128, 1024], F32)
        xs = sga
        xs2 = sugv
        alT = sb("alT", [32, 128], F32)
        Lg = sb("Lg", [128, 512], F32)
        Ek = sb("Ek", [128, 512], F32)
        dec = sb("dec", [128, 8], F32)
        kdec = sb("kdec", [128, 512], BF16)
        ya = Lg
        ya_bf = sb("ya_bf", [128, 512], BF16)
        yaT = sb("yaT", [128, 4, 128], BF16)
        vn = Ek
        vn_bf = sb("vn_bf", [128, 512], BF16)
        yb = sb("yb", [128, 512], BF16)
        ybT = sb("ybT", [128, 4, 128], BF16)
        mgq = sb("mgq", [128, 2048], BF16)
        mg_bf = mgq[:, 0:1024]
        mgT = mgq[:, 1024:2048].rearrange("p (c t) -> p c t", c=8)
        qpT = mgq[:].rearrange("p (g t) -> p g t", g=16)
        n2 = [sb(f"n2_{i}", [128, 1024], F32) for i in range(2)]
        s_sb = sb("s_sb", [128, 16, 128], F32)
        V16 = sb("V16", [128, 16, 16], F32)
        I16 = sb("I16", [128, 16, 16], U32)
        VAL = sb("VAL", [128, 8, 16], F32)
        CI = sb("CI", [128, 8, 16], U32)
        IA = sb("IA", [128, 8, 16], U32)
        IB = sb("IB", [128, 8, 16], U32)
        IAf = sb("IAf", [128, 8, 16], F32)
        IBf = sb("IBf", [128, 8, 16], F32)
        E1 = sb("E1", [128, 8, 16], F32)
        E2 = sb("E2", [128, 8, 16], F32)
        idxf = sb("idxf", [128, 128], F32)
        idx = [sb(f"idx{i}", [128, 128], I32) for i in range(2)]
        ge = sb("ge", [128, 8, 16], F32)
        gs = sb("gs", [128, 8], F32)
        gates = [sb(f"gates{i}", [128, 128], F32) for i in range(2)]
        hh = sb("hh", [128, 128], F32)
        gl = sb("gl", [128, 128], F32)
        aw = sb("aw", [128, 128], F32)
        sm1 = sb("sm1", [128, 2], F32)
        smo = sb("smo", [128, 8], F32)
        smln = sb("smln", [128, 1], F32)
        sm2 = sb("sm2", [128, 2], F32)
        sm3 = sb("sm3", [128, 2], F32)
        ghalf = sb("ghalf", [128, 4], F32)
        bnst = sb("bnst", [128, 6], F32)
        bnag = sb("bnag", [128, 2], F32)

        tp = ps("tp", [128, 1024], BF16)
        pacc = [ps(f"pacc{i}", [128, 512], F32) for i in range(2)]
        NPB = 5
        pbank = [ps(f"pb{i}", [128, 512], F32) for i in range(NPB)]
        pctr = [0]

        def bank():
            i = pctr[0] % NPB
            pctr[0] += 1
            return pbank[i], f"pb{i}"

        def cap(name):
            a, b = CA[name]
            return cA[:, a:b]

        S.dma("sp", lambda h: h.dma_start(out=cA[:], in_=cA_d), "ldc", writes=["cA"])
        stg = ring[:].bitcast(F32)
        S.dma("sp", lambda h: h.dma_start(out=stg[:, 0:NCB], in_=cB_d), "ldc2", writes=["stg0"])

        def cbp(name):
            a, b = CB[name]
            return stg[:, a:b]

        S.op("dve", lambda h: h.memset(alT[:], 1.0), writes=["alT"])
        S.op("dve", lambda h: h.tensor_scalar(out=ghalf[:], in0=cap("goutT"), scalar1=0.5, scalar2=None, op0=ALU.mult),
             reads=["cA"], writes=["ghalf"])
        S.op("dve", lambda h: h.memset(state[:], 0.0), writes=["state"])
        S.op("dve", lambda h: h.tensor_copy(out=ident[:], in_=cbp("ident")), reads=["stg0"], writes=["ident"])
        S.op("dve", lambda h: h.tensor_copy(out=k1T[:], in_=cbp("k1T")), reads=["stg0"], writes=["k1T"])
        S.op("dve", lambda h: h.tensor_copy(out=k2T[:], in_=cbp("k2T")), reads=["stg0"], writes=["k2T"])
        S.op("dve", lambda h: h.tensor_copy(out=walr[:].rearrange("p c n -> p (c n)"), in_=cbp("walr")),
             reads=["stg0"], writes=["walr"])
        msk = cbp("mask")
        S.op("dve", lambda h: h.tensor_tensor(
            out=swT[:], in0=cbp("swT").rearrange("p (g i) -> p g i", g=4),
            in1=msk.unsqueeze(1).to_broadcast([128, 4, 128]), op=ALU.mult),
            reads=["stg0"], writes=["swT"])
        for b in range(NBLK):
            S.dma("pool", lambda h, b=b: h.dma_start(out=wsc_d[b], in_=wall_d[b]), f"wcv{b}", writes=[f"wsc{b}"])
        S.dma("pool", lambda h: h.dma_start(out=wa[:].rearrange("p c n -> p (c n)"), in_=wall_d[14]), "wcva", writes=["wa"])
        S.dma("pool", lambda h: h.dma_start(out=wb[:].rearrange("p c n -> p (c n)"), in_=wall_d[15]), "wcvb", writes=["wb"])
        for hb in range(2):
            S.dma("pool", lambda h, hb=hb: h.dma_start(
                out=wo[:, hb * 4:hb * 4 + 4, :].rearrange("p c n -> p (c n)"), in_=wall_d[16 + hb]),
                f"wcvo{hb}", writes=["wo"])
        NCH = 32
        RCH = NEXP // NCH
        TBL_KEYS = [f"tbl{i}" for i in range(NCH)]
        for i in range(NCH):
            S.dma("pool", lambda h, i=i: h.dma_start(out=tbl_d[i * RCH:(i + 1) * RCH, :],
                                                     in_=puv_d[i * RCH:(i + 1) * RCH, :]),
                  "tcv", writes=[TBL_KEYS[i]])

        wctr = [0]
        rctr = [0]
        dctr = [0]
        ring_alias = {b: [f"stg{min(b * 2048 * 2 // (4096 * 4), 1)}"] for b in range(NR)}

        def build_AB(tt):
            Q = Defer()
            p = tt % 2
            ACC, ACCk = acc[p], f"acc{p}"
            N2, N2k = n2[p], f"n2_{p}"
            IDX, IDXk = idx[p], f"idx{p}"
            GT, GTk = gates[p], f"gates{p}"
            r0 = tt * 128
            T0 = (tt == 0)

            def tap(name, ap, key, shape):
                if name not in taps or not T0:
                    return
                t = nc.dram_tensor("tap_" + name, list(shape), ap.dtype, kind="ExternalOutput").ap()
                tap_d[name] = t
                Q.dma("sp", lambda h: h.dma_start(out=t, in_=ap), "tap_" + name, reads=[key])

            def rstd_from_ss(ss_ap, out_ap, n, key):
                Q.op("act", lambda h: h.activation(out=out_ap, in_=ss_ap, func=AF.Ln, scale=1.0 / n, bias=EPS),
                     reads=[key], writes=[key])
                Q.op("act", lambda h: h.activation(out=out_ap, in_=out_ap, func=AF.Exp, scale=-0.5),
                     reads=[key], writes=[key])

            def transposes(src_tile, nchunks, srckey):
                for c in range(nchunks):
                    Q.op("pe", lambda h, c=c: h.transpose(out=tp[:, c * 128:(c + 1) * 128],
                                                         in_=src_tile[:, c * 128:(c + 1) * 128], identity=ident[:]),
                         reads=[srckey, "ident"], writes=["tp"])

            def wload(blk):
                i = wctr[0] % NWB
                wctr[0] += 1
                Q.dma("sp", lambda h, i=i, blk=blk: h.dma_start(out=wbuf[i][:].rearrange("p c n -> p (c n)"),
                                                                in_=wsc_d[blk]),
                      f"wl{i}", reads=[f"wsc{blk}"], writes=[f"wbuf{i}"])
                return wbuf[i], f"wbuf{i}"

            if tt == 0:
                Q.dma("sp", lambda h: h.dma_start(out=xs[:], in_=x_d[0:128, :]), "ldx", writes=["sga"])
            Q.op("act", lambda h: h.activation(out=junk_a[:], in_=xs[:], func=AF.Square, accum_out=sm1[:, 0:1]),
                 reads=["sga"], writes=["junk_a", "sm1"])
            rstd_from_ss(sm1[:, 0:1], sm1[:, 1:2], D, "sm1")
            Q.op("act", lambda h: h.activation(out=xn[:], in_=xs[:], func=AF.Copy, scale=sm1[:, 1:2]),
                 reads=["sga", "sm1"], writes=["xn"])
            transposes(xn, 8, "xn")
            Q.op("dve", lambda h: h.tensor_tensor(out=nT[:], in0=tp[:].rearrange("p (c t) -> p c t", c=8),
                                                  in1=cap("g1T").unsqueeze(2).to_broadcast([128, 8, 128]),
                                                  op=ALU.mult),
                 reads=["tp", "cA"], writes=["nT"])

            w, wk = wload(0)
            pq, pqk = bank()
            for hd in range(4):
                for c in range(8):
                    Q.op("pe", lambda h, hd=hd, c=c, w=w, pq=pq: h.matmul(
                        out=pq[:, hd * 128:(hd + 1) * 128], lhsT=w[:, c, hd * 128:(hd + 1) * 128],
                        rhs=nT[:, c, :], start=(c == 0), stop=(c == 7)),
                        reads=[wk, "nT"], writes=[pqk])
            Q.op("act", lambda h, pq=pq: h.activation(out=qT[:], in_=pq[:], func=AF.Copy, scale=128.0 ** -0.5),
                 reads=[pqk], writes=["qT"])

            def proj_block(blk):
                w, wk = wload(blk)
                pb, pbk = bank()
                for c in range(8):
                    Q.op("pe", lambda h, c=c, w=w, pb=pb: h.matmul(out=pb[:], lhsT=nT[:, c, :], rhs=w[:, c, :],
                                                                  start=(c == 0), stop=(c == 7)),
                         reads=[wk, "nT"], writes=[pbk])
                return pb, pbk

            pa, pak = bank()
            for c in range(8):
                Q.op("pe", lambda h, c=c, pa=pa: h.matmul(out=pa[0:16, 0:128], lhsT=walr[:, c, :], rhs=nT[:, c, :],
                                                         start=(c == 0), stop=(c == 7)),
                     reads=["walr", "nT"], writes=[pak])
            Q.op("act", lambda h, pa=pa: h.activation(out=alT[0:16, :], in_=pa[0:16, 0:128], func=AF.Copy),
                 reads=[pak], writes=["alT"])
            pz, pzk = bank()
            Q.op("pe", lambda h, pz=pz: h.matmul(out=pz[:], lhsT=alT[0:17, :], rhs=cap("wga")[0:17, :],
                                                 start=True, stop=True),
                 reads=["alT", "cA"], writes=[pzk])
            Q.op("act", lambda h, pz=pz: h.activation(out=Lg[:], in_=pz[:], func=AF.Exp, scale=-1.0),
                 reads=[pzk], writes=["Lg"])
            Q.op("act", lambda h: h.activation(out=Lg[:], in_=Lg[:], func=AF.Ln, bias=1.0, scale=1.0),
                 reads=["Lg"], writes=["Lg"])
            pb, pbk = proj_block(1)
            Q.op("act", lambda h, pb=pb: h.activation(out=k_sb[:], in_=pb[:], func=AF.Copy), reads=[pbk], writes=["k_sb"])
            pb, pbk = proj_block(2)
            Q.op("act", lambda h, pb=pb: h.activation(out=v_bf[:], in_=pb[:], func=AF.Copy), reads=[pbk], writes=["v_bf"])
            prv, prvk = bank()
            Q.op("pe", lambda h, prv=prv: h.matmul(out=prv[:], lhsT=cap("trirev"), rhs=Lg[:], start=True, stop=True),
                 reads=["cA", "Lg"], writes=[prvk])
            ptt, pttk = bank()
            for hd in range(4):
                Q.op("pe", lambda h, hd=hd, ptt=ptt: h.matmul(out=ptt[:, hd * 2:hd * 2 + 2],
                                                              lhsT=Lg[:, hd * 128:(hd + 1) * 128],
                                                              rhs=cap("chunkind"), start=True, stop=True),
                     reads=["cA", "Lg"], writes=[pttk])
            Q.op("act", lambda h, prv=prv: h.activation(out=Ek[:], in_=prv[:], func=AF.Exp), reads=[prvk], writes=["Ek"])
            Q.op("act", lambda h, ptt=ptt: h.activation(out=dec[:], in_=ptt[:, 0:8], func=AF.Exp), reads=[pttk], writes=["dec"])
            Q.op("dve", lambda h: h.tensor_tensor(out=kdec[:], in0=k_sb[:], in1=Ek[:], op=ALU.mult),
                 reads=["k_sb", "Ek"], writes=["kdec"])
            pb, pbk = proj_block(3)
            Q.op("act", lambda h, pb=pb: h.activation(out=silu_r[:], in_=pb[:], func=AF.Tanh, scale=0.5), reads=[pbk], writes=["silu_r"])
            Q.op("dve", lambda h, pb=pb: h.scalar_tensor_tensor(out=silu_r[:], in0=silu_r[:], scalar=1.0, in1=pb[:],
                                                                op0=ALU.add, op1=ALU.mult),
                 reads=[pbk, "silu_r"], writes=["silu_r"])
            for i, blk in enumerate((4, 5)):
                pb, pbk = proj_block(blk)
                Q.op("act", lambda h, pb=pb, i=i: h.activation(out=sugv[:, i * 512:(i + 1) * 512], in_=pb[:],
                                                               func=AF.Gelu_apprx_tanh),
                     reads=[pbk], writes=["sugv"])
            for i, blk in enumerate((6, 7)):
                pb, pbk = proj_block(blk)
                Q.op("act", lambda h, pb=pb, i=i: h.activation(out=sga[:, i * 512:(i + 1) * 512], in_=pb[:], func=AF.Tanh, scale=0.5),
                     reads=[pbk], writes=["sga"])
            for i, blk in enumerate((8, 9)):
                pb, pbk = proj_block(blk)
                Q.op("act", lambda h, pb=pb, i=i: h.activation(out=sgb[:, i * 512:(i + 1) * 512], in_=pb[:], func=AF.Tanh, scale=0.5),
                     reads=[pbk], writes=["sgb"])

            po, pok = bank()
            for j in range(2):
                pkv, pkvk = bank()
                for hd in range(4):
                    Q.op("pe", lambda h, hd=hd, j=j, pkv=pkv: h.matmul(
                        out=pkv[:, hd * 128:(hd + 1) * 128],
                        lhsT=kdec[64 * j:64 * j + 64, hd * 128:(hd + 1) * 128],
                        rhs=v_bf[64 * j:64 * j + 64, hd * 128:(hd + 1) * 128], start=True, stop=True),
                        reads=["kdec", "v_bf"], writes=[pkvk])
                for hd in range(4):
                    Q.op("dve", lambda h, hd=hd, j=j, pkv=pkv: h.scalar_tensor_tensor(
                        out=state[:, hd * 128:(hd + 1) * 128], in0=state[:, hd * 128:(hd + 1) * 128],
                        scalar=dec[:, hd * 2 + j:hd * 2 + j + 1], in1=pkv[:, hd * 128:(hd + 1) * 128],
                        op0=ALU.mult, op1=ALU.add),
                        reads=[f"state{hd}", "dec", pkvk], writes=[f"state{hd}"])
                Q.op("act", lambda h, j=j: h.activation(out=state_bf[j][:], in_=state[:], func=AF.Copy),
                     reads=[f"state{hd}" for hd in range(4)], writes=[f"state_bf{j}"])
                for hd in range(4):
                    Q.op("pe", lambda h, hd=hd, j=j, po=po: h.matmul(
                        out=po[64 * j:64 * j + 64, hd * 128:(hd + 1) * 128],
                        lhsT=qT[:, hd * 128 + 64 * j:hd * 128 + 64 * j + 64],
                        rhs=state_bf[j][:, hd * 128:(hd + 1) * 128], start=True, stop=True),
                        reads=["qT", f"state_bf{j}"], writes=[pok])
            for hd in range(4):
                Q.op("act", lambda h, hd=hd, po=po: h.activation(out=junk_a[:, hd * 128:(hd + 1) * 128],
                                                                 in_=po[:, hd * 128:(hd + 1) * 128], func=AF.Square,
                                                                 accum_out=smo[:, hd:hd + 1]),
                     reads=[pok], writes=["junk_a", "smo"])
            rstd_from_ss(smo[:, 0:4], smo[:, 4:8], 128, "smo")
            Q.op("dve", lambda h, po=po: h.tensor_tensor(out=ya[:].rearrange("p (g v) -> p g v", g=4),
                                                         in0=po[:].rearrange("p (g v) -> p g v", g=4),
                                                         in1=smo[:, 4:8].unsqueeze(2).to_broadcast([128, 4, 128]),
                                                         op=ALU.mult),
                 reads=[pok, "smo"], writes=["Lg"])
            Q.op("dve", lambda h: h.tensor_tensor(out=ya_bf[:], in0=ya[:], in1=silu_r[:], op=ALU.mult),
                 reads=["Lg", "silu_r"], writes=["ya_bf"])
            transposes(ya_bf, 4, "ya_bf")
            Q.op("dve", lambda h: h.tensor_tensor(out=yaT[:], in0=tp[:, 0:512].rearrange("p (c t) -> p c t", c=4),
                                                  in1=ghalf[:].unsqueeze(2).to_broadcast([128, 4, 128]),
                                                  op=ALU.mult),
                 reads=["tp", "ghalf"], writes=["yaT"])

            svg = sugv[:, 512:1024]
            Q.op("dve", lambda h: h.bn_stats(out=bnst[:], in_=svg), reads=["sugv"], writes=["bnst"])
            Q.op("dve", lambda h: h.bn_aggr(out=bnag[:], in_=bnst[:]), reads=["bnst"], writes=["bnag"])
            Q.op("act", lambda h: h.activation(out=smln[:], in_=bnag[:, 1:2], func=AF.Ln, scale=1.0, bias=EPS),
                 reads=["bnag"], writes=["smln"])
            Q.op("act", lambda h: h.activation(out=smln[:], in_=smln[:], func=AF.Exp, scale=-0.5),
                 reads=["smln"], writes=["smln"])
            Q.op("dve", lambda h: h.tensor_scalar(out=vn[:], in0=svg, scalar1=bnag[:, 0:1], scalar2=smln[:, 0:1],
                                                  op0=ALU.subtract, op1=ALU.mult),
                 reads=["sugv", "bnag", "smln"], writes=["Ek"])
            Q.op("dve", lambda h: h.tensor_tensor(out=vn[:], in0=vn[:], in1=cap("lng"), op=ALU.mult),
                 reads=["Ek", "cA"], writes=["Ek"])
            Q.op("dve", lambda h: h.tensor_tensor(out=vn_bf[:], in0=vn[:], in1=cap("lnb"), op=ALU.add),
                 reads=["Ek", "cA"], writes=["vn_bf"])
            pm, pmk = bank()
            for g in range(4):
                Q.op("pe", lambda h, g=g, pm=pm: h.matmul(out=pm[:, g * 128:(g + 1) * 128], lhsT=swT[:, g, :],
                                                         rhs=vn_bf[:, g * 128:(g + 1) * 128], start=True, stop=True),
                     reads=["swT", "vn_bf"], writes=[pmk])
            for g in range(4):
                Q.op("dve", lambda h, g=g, pm=pm: h.scalar_tensor_tensor(
                    out=yb[:, g * 128:(g + 1) * 128], in0=pm[:, g * 128:(g + 1) * 128],
                    scalar=cap("sbT")[:, g:g + 1], in1=sugv[:, g * 128:(g + 1) * 128], op0=ALU.add, op1=ALU.mult),
                    reads=[pmk, "cA", "sugv"], writes=["yb"])
            transposes(yb, 4, "yb")
            Q.dma("sp", lambda h: h.dma_start(out=xs2[:], in_=x_d[r0:r0 + 128, :]), "ldx2", writes=["sugv"])
            Q.op("act", lambda h: h.activation(out=ybT[:].rearrange("p c t -> p (c t)"), in_=tp[:, 0:512], func=AF.Copy),
                 reads=["tp"], writes=["ybT"])

            for half in range(2):
                pA, pAk = bank()
                for c in range(4):
                    Q.op("pe", lambda h, c=c, half=half, pA=pA: h.matmul(
                        out=pA[:], lhsT=yaT[:, c, :], rhs=wa[:, c, half * 512:(half + 1) * 512],
                        start=(c == 0), stop=(c == 3)), reads=["yaT", "wa"], writes=[pAk])
                Q.op("dve", lambda h, half=half, pA=pA: h.scalar_tensor_tensor(
                    out=sga[:, half * 512:(half + 1) * 512], in0=sga[:, half * 512:(half + 1) * 512], scalar=1.0,
                    in1=pA[:], op0=ALU.add, op1=ALU.mult), reads=[pAk, "sga"], writes=["sga"])
                pB, pBk = bank()
                for c in range(4):
                    Q.op("pe", lambda h, c=c, half=half, pB=pB: h.matmul(
                        out=pB[:], lhsT=ybT[:, c, :], rhs=wb[:, c, half * 512:(half + 1) * 512],
                        start=(c == 0), stop=(c == 3)), reads=["ybT", "wb"], writes=[pBk])
                Q.op("dve", lambda h, half=half, pB=pB: h.scalar_tensor_tensor(
                    out=sgb[:, half * 512:(half + 1) * 512], in0=sgb[:, half * 512:(half + 1) * 512], scalar=1.0,
                    in1=pB[:], op0=ALU.add, op1=ALU.mult), reads=[pBk, "sgb"], writes=["sgb"])
            Q.op("dve", lambda h: h.tensor_tensor(out=mg_bf, in0=sga[:], in1=sgb[:], op=ALU.add),
                 reads=["sga", "sgb"], writes=["mgq"])
            transposes(mg_bf, 8, "mgq")
            Q.op("act", lambda h: h.activation(out=mgq[:, 1024:2048], in_=tp[:], func=AF.Copy, scale=0.5),
                 reads=["tp"], writes=["mgq"])
            for half in range(2):
                pD, pDk = bank()
                for c in range(8):
                    Q.op("pe", lambda h, c=c, half=half, pD=pD: h.matmul(
                        out=pD[:], lhsT=mgT[:, c, :], rhs=wo[:, c, half * 512:(half + 1) * 512],
                        start=(c == 0), stop=(c == 7)), reads=["mgq", "wo"], writes=[pDk])
                Q.op("dve", lambda h, half=half, pD=pD: h.tensor_tensor(
                    out=ACC[:, half * 512:(half + 1) * 512], in0=pD[:], in1=xs2[:, half * 512:(half + 1) * 512],
                    op=ALU.add), reads=[pDk, "sugv"], writes=[ACCk])
            tap("h", ACC[:], ACCk, [128, 1024])
            if tt + 1 < NT:
                Q.dma("sp", lambda h: h.dma_start(out=xs[:], in_=x_d[r0 + 128:r0 + 256, :]), "ldx", writes=["sga"])

            Q.op("act", lambda h: h.activation(out=junk_a[:], in_=ACC[:], func=AF.Square, accum_out=sm2[:, 0:1]),
                 reads=[ACCk], writes=["junk_a", "sm2"])
            rstd_from_ss(sm2[:, 0:1], sm2[:, 1:2], D, "sm2")
            Q.op("dve", lambda h: h.scalar_tensor_tensor(out=N2[:], in0=ACC[:], scalar=sm2[:, 1:2], in1=cap("g2bc"),
                                                         op0=ALU.mult, op1=ALU.mult),
                 reads=[ACCk, "sm2", "cA"], writes=[N2k])
            Q.op("act", lambda h: h.activation(out=xn[:], in_=N2[:], func=AF.Copy), reads=[N2k], writes=["xn"])
            transposes(xn, 8, "xn")
            Q.op("act", lambda h: h.activation(out=nT[:].rearrange("p c t -> p (c t)"), in_=tp[:], func=AF.Copy),
                 reads=["tp"], writes=["nT"])

            for blk in range(4):
                w, wk = wload(10 + blk)
                pb, pbk = bank()
                for gg in range(4):
                    for c in range(8):
                        Q.op("pe", lambda h, gg=gg, c=c, w=w, pb=pb: h.matmul(
                            out=pb[:, gg * 128:(gg + 1) * 128], lhsT=w[:, c, gg * 128:(gg + 1) * 128],
                            rhs=nT[:, c, :], start=(c == 0), stop=(c == 7)),
                            reads=[wk, "nT"], writes=[pbk])
                Q.op("act", lambda h, blk=blk, pb=pb: h.activation(
                    out=qpT[:, blk * 4:(blk + 1) * 4, :].rearrange("p g t -> p (g t)"), in_=pb[:], func=AF.Copy),
                    reads=[pbk], writes=["mgq"])
            for blk in range(4):
                pb, pbk = bank()
                for gg in range(4):
                    g = blk * 4 + gg
                    kk, kkk = (k1T, "k1T") if g % 2 == 0 else (k2T, "k2T")
                    Q.op("pe", lambda h, g=g, gg=gg, kk=kk, pb=pb: h.matmul(
                        out=pb[:, gg * 128:(gg + 1) * 128], lhsT=qpT[:, g, :], rhs=kk[:], start=True, stop=True),
                        reads=["mgq", kkk], writes=[pbk])
                Q.op("act", lambda h, blk=blk, pb=pb: h.activation(
                    out=s_sb[:, blk * 4:(blk + 1) * 4, :].rearrange("p g t -> p (g t)"), in_=pb[:], func=AF.Copy),
                    reads=[pbk], writes=["s_sb"])

            for g in range(16):
                sg = s_sb[:, g, :]
                Q.op("dve", lambda h, g=g, sg=sg: h.max(out=V16[:, g, 0:8], in_=sg), reads=["s_sb"], writes=["V16"])
                Q.op("dve", lambda h, g=g, sg=sg: h.max_index(out=I16[:, g, 0:8], in_max=V16[:, g, 0:8], in_values=sg),
                     reads=["s_sb", "V16"], writes=["I16"])
                Q.op("dve", lambda h, g=g, sg=sg: h.match_replace(out=s_wk[:, 0:128], in_to_replace=V16[:, g, 0:8],
                                                                  in_values=sg, imm_value=-1e30),
                     reads=["s_sb", "V16"], writes=["s_wk"])
                Q.op("dve", lambda h, g=g: h.max(out=V16[:, g, 8:16], in_=s_wk[:, 0:128]), reads=["s_wk"], writes=["V16"])
                Q.op("dve", lambda h, g=g: h.max_index(out=I16[:, g, 8:16], in_max=V16[:, g, 8:16],
                                                       in_values=s_wk[:, 0:128]),
                     reads=["s_wk", "V16"], writes=["I16"])
            Q.op("dve", lambda h: h.tensor_copy(out=I16[:].bitcast(F32), in_=I16[:]), reads=["I16"], writes=["I16"])
            V16v = V16[:].rearrange("p (h t) r -> p h t r", t=2)
            I16v = I16[:].bitcast(F32).rearrange("p (h t) r -> p h t r", t=2)
            grid = s_sb[:].rearrange("p g n -> p (g n)").rearrange("p (h a b) -> p h a b", h=8, a=16)
            Q.op("dve", lambda h: h.tensor_tensor(out=grid, in0=V16v[:, :, 0, :].unsqueeze(3).to_broadcast([128, 8, 16, 16]),
                                                  in1=V16v[:, :, 1, :].unsqueeze(2).to_broadcast([128, 8, 16, 16]),
                                                  op=ALU.add),
                 reads=["V16", "s_sb"], writes=["s_sb"])
            candf = s_sb[:].rearrange("p g n -> p (g n)").rearrange("p (h c) -> p h c", h=8)
            for hd in range(8):
                ch = candf[:, hd, :]
                Q.op("dve", lambda h, hd=hd, ch=ch: h.max(out=VAL[:, hd, 0:8], in_=ch), reads=["s_sb"], writes=["VAL"])
                Q.op("dve", lambda h, hd=hd, ch=ch: h.max_index(out=CI[:, hd, 0:8], in_max=VAL[:, hd, 0:8], in_values=ch),
                     reads=["s_sb", "VAL"], writes=["CI"])
                Q.op("dve", lambda h, hd=hd, ch=ch: h.match_replace(out=s_wk, in_to_replace=VAL[:, hd, 0:8],
                                                                    in_values=ch, imm_value=-1e30),
                     reads=["s_sb", "VAL"], writes=["s_wk"])
                Q.op("dve", lambda h, hd=hd: h.max(out=VAL[:, hd, 8:16], in_=s_wk), reads=["s_wk"], writes=["VAL"])
                Q.op("dve", lambda h, hd=hd: h.max_index(out=CI[:, hd, 8:16], in_max=VAL[:, hd, 8:16], in_values=s_wk),
                     reads=["s_wk", "VAL"], writes=["CI"])
            Q.op("dve", lambda h: h.tensor_single_scalar(out=IA[:], in_=CI[:], scalar=4, op=ALU.logical_shift_right),
                 reads=["CI"], writes=["IA"])
            Q.op("dve", lambda h: h.tensor_single_scalar(out=IB[:], in_=CI[:], scalar=15, op=ALU.bitwise_and),
                 reads=["CI"], writes=["IB"])
            Q.op("dve", lambda h: h.tensor_copy(out=IAf[:], in_=IA[:]), reads=["IA"], writes=["IAf"])
            Q.op("dve", lambda h: h.tensor_copy(out=IBf[:], in_=IB[:]), reads=["IB"], writes=["IBf"])
            iob = cap("iota16").unsqueeze(1).unsqueeze(1).to_broadcast([128, 8, 16, 16])
            for (If, Ifk, half, Eo, Ek_) in ((IAf, "IAf", 0, E1, "E1"), (IBf, "IBf", 1, E2, "E2")):
                Q.op("dve", lambda h, If=If: h.tensor_tensor(out=grid, in0=If[:].unsqueeze(3).to_broadcast([128, 8, 16, 16]),
                                                             in1=iob, op=ALU.is_equal),
                     reads=[Ifk, "cA"], writes=["s_sb"])
                Q.op("dve", lambda h, half=half: h.tensor_tensor(
                    out=grid, in0=grid, in1=I16v[:, :, half, :].unsqueeze(2).to_broadcast([128, 8, 16, 16]), op=ALU.mult),
                    reads=["s_sb", "I16"], writes=["s_sb"])
                Q.op("dve", lambda h, Eo=Eo: h.tensor_reduce(out=Eo[:], in_=grid, axis=AX.X, op=ALU.add),
                     reads=["s_sb"], writes=[Ek_])
            Q.op("dve", lambda h: h.scalar_tensor_tensor(out=idxf[:], in0=E1[:].rearrange("p h k -> p (h k)"), scalar=128.0,
                                                         in1=E2[:].rearrange("p h k -> p (h k)"), op0=ALU.mult, op1=ALU.add),
                 reads=["E1", "E2"], writes=["idxf"])
            Q.op("dve", lambda h: h.tensor_copy(out=IDX[:], in_=idxf[:]), reads=["idxf"], writes=[IDXk])
            Q.op("dve", lambda h: h.tensor_tensor(out=ge[:], in0=VAL[:], in1=VAL[:, :, 0:1].to_broadcast([128, 8, 16]),
                                                  op=ALU.subtract), reads=["VAL"], writes=["ge"])
            Q.op("act", lambda h: h.activation(out=ge[:], in_=ge[:], func=AF.Exp), reads=["ge"], writes=["ge"])
            Q.op("dve", lambda h: h.tensor_reduce(out=gs[:], in_=ge[:], axis=AX.X, op=ALU.add), reads=["ge"], writes=["gs"])
            Q.op("dve", lambda h: h.reciprocal(out=gs[:], in_=gs[:]), reads=["gs"], writes=["gs"])
            Q.op("dve", lambda h: h.tensor_tensor(out=GT[:].rearrange("p (h k) -> p h k", h=8), in0=ge[:],
                                                  in1=gs[:].unsqueeze(2).to_broadcast([128, 8, 16]), op=ALU.mult),
                 reads=["ge", "gs"], writes=[GTk])
            tap("idxf", idxf[:], "idxf", [128, 128])
            tap("gates", GT[:], GTk, [128, 128])
            return Q.items

        def run_items(items, lo, hi):
            for kind, a, k in items[lo:hi]:
                getattr(S, kind)(*a, **k)

        GRP = 2
        X_LAT = 1.0
        PE_LAT = 2.0
        DMA_LAT = 5.0
        BUDGET = 3
        EARLY = 100
        TAIL_DELAY = 8.0
        TOPK_KEYS = {"s_sb", "V16", "I16", "s_wk", "I16f", "VAL", "CI", "IA", "IB", "IAf", "IBf", "E1", "E2",
                     "idxf", "ge", "gs"}

        pending = deque()
        G = [0]
        floor_t = [0.0]

        def lat_from(eng):
            return {"dma": DMA_LAT, "pe": PE_LAT}.get(eng, X_LAT)

        tails = deque()
        gcnt = {}

        def enqueue_AB(t, base):
            items = build_AB(t)
            i_tail = len(items)
            for i, it in enumerate(items):
                if it[0] == "op" and it[1][0] == "dve" and "V16" in it[2].get("writes", ()):
                    i_tail = i
                    break
            acck = f"acc{t % 2}"
            barrier = float((t - 1) * 128)
            lw = {}
            lr = {}
            times = []
            t0 = max(float(base), floor_t[0])
            for it in items:
                eng = it[1][0] if it[0] == "op" else "dma"
                kk = it[2]
                reads, writes = kk.get("reads", ()), kk.get("writes", ())
                ready = t0
                if acck in writes:
                    ready = max(ready, barrier)
                for key in list(reads) + list(writes):
                    w = lw.get(key)
                    if w is not None:
                        ready = max(ready, w[1] + (lat_from(w[0]) if w[0] != eng else 0.0))
                for key in writes:
                    for (re_, rt_) in lr.get(key, ()):
                        ready = max(ready, rt_ + (lat_from(re_) if re_ != eng else 0.0))
                if eng == "dve":
                    while gcnt.get(int(ready), 0) >= BUDGET:
                        ready = float(int(ready) + 1)
                    gcnt[int(ready)] = gcnt.get(int(ready), 0) + 1
                times.append(ready)
                for key in reads:
                    lr.setdefault(key, []).append((eng, ready))
                for key in writes:
                    lw[key] = (eng, ready)
                    lr[key] = []
            order = sorted(range(i_tail), key=lambda i: (times[i], i))
            for i in order:
                pending.append((times[i], t, items[i]))
            order = sorted(range(i_tail, len(items)), key=lambda i: (times[i], i))
            for i in order:
                tails.append((times[i] + TAIL_DELAY, t, items[i]))
            floor_t[0] = max(times[:i_tail]) if i_tail else t0

        def emit_item(it):
            getattr(S, it[0])(*it[1], **it[2])

        def pop_ready(limit_tile=None):
            while True:
                progressed = False
                if pending and (pending[0][0] <= G[0] if limit_tile is None else pending[0][1] <= limit_tile):
                    it = pending[0][2]
                    if any(k_ in TOPK_KEYS for k_ in it[2].get("writes", ())):
                        while tails and tails[0][1] < pending[0][1]:
                            emit_item(tails.popleft()[2])
                    emit_item(pending.popleft()[2])
                    progressed = True
                if tails and (tails[0][0] <= G[0] if limit_tile is None else tails[0][1] <= limit_tile):
                    if not (pending and pending[0][1] <= tails[0][1]):
                        emit_item(tails.popleft()[2])
                        progressed = True
                if not progressed:
                    break

        def emit_CD(tt):
            p = tt % 2
            ACC, ACCk = acc[p], f"acc{p}"
            N2, N2k = n2[p], f"n2_{p}"
            IDX, IDXk = idx[p], f"idx{p}"
            GT, GTk = gates[p], f"gates{p}"
            r0 = tt * 128
            G[0] = tt * 128

            def advance():
                pop_ready()
                G[0] += 1

            for gi in range(128 // GRP):
                slots = []
                for jj in range(GRP):
                    j = gi * GRP + jj
                    b = rctr[0] % NR
                    rctr[0] += 1
                    slots.append((j, b))
                    rs = ring[:, b * 2048:(b + 1) * 2048]
                    S.dma("pool", lambda h, rs=rs, j=j: h.indirect_dma_start(
                        out=rs, out_offset=None, in_=tbl_d,
                        in_offset=bass.IndirectOffsetOnAxis(ap=IDX[:, j:j + 1], axis=0)),
                        f"g{b}", reads=([IDXk] + TBL_KEYS) if tt == 0 and j == 0 else [IDXk],
                        writes=[f"ring{b}", "stg0", "stg1"] if tt == 0 and j < NR else [f"ring{b}"])
                    S.op("dve", lambda h, rs=rs, j=j: h.scalar_tensor_tensor(
                        out=junk_d, in0=rs[:, 0:1024], scalar=1.0, in1=N2[:], op0=ALU.mult, op1=ALU.mult,
                        accum_out=hh[:, j:j + 1]),
                        reads=[f"ring{b}", N2k], writes=["jd", f"hh{gi}"])
                    advance()
                j0 = gi * GRP
                S.op("act", lambda h, j0=j0: h.activation(out=gl[:, j0:j0 + GRP], in_=hh[:, j0:j0 + GRP],
                                                          func=AF.Gelu_apprx_tanh), reads=[f"hh{gi}"], writes=["gl"])
                for (j, b) in slots:
                    di = dctr[0] % NDG
                    dctr[0] += 1
                    S.op("act", lambda h, j=j: h.activation(out=aw[:, j:j + 1], in_=gl[:, j:j + 1], func=AF.Copy,
                                                            scale=GT[:, j:j + 1]),
                         reads=["gl", GTk], writes=["aw"])
                    S.op("act", lambda h, j=j, di=di: h.activation(out=dg[:, di, :], in_=ident[:], func=AF.Copy,
                                                                   scale=aw[:, j:j + 1]),
                         reads=["ident", "aw"], writes=[f"dg{di}"])
                    for half in range(2):
                        S.op("pe", lambda h, j=j, b=b, di=di, half=half: h.matmul(
                            out=pacc[half][:], lhsT=dg[:, di, :],
                            rhs=ring[:, b * 2048 + 1024 + half * 512:b * 2048 + 1024 + (half + 1) * 512],
                            start=(j == 0), stop=(j == 127)),
                            reads=[f"dg{di}", f"ring{b}"], writes=[f"pacc{half}"])
            pop_ready(limit_tile=tt + 1)
            for half in range(2):
                S.op("dve", lambda h, half=half: h.tensor_tensor(
                    out=ACC[:, half * 512:(half + 1) * 512], in0=pacc[half][:], in1=ACC[:, half * 512:(half + 1) * 512],
                    op=ALU.add), reads=[f"pacc{half}", ACCk], writes=[ACCk])
            S.op("act", lambda h: h.activation(out=junk_a[:], in_=ACC[:], func=AF.Square, accum_out=sm3[:, 0:1]),
                 reads=[ACCk], writes=["junk_a", "sm3"])
            S.op("act", lambda h: h.activation(out=sm3[:, 1:2], in_=sm3[:, 0:1], func=AF.Ln, scale=1.0 / D, bias=EPS),
                 reads=["sm3"], writes=["sm3"])
            S.op("act", lambda h: h.activation(out=sm3[:, 1:2], in_=sm3[:, 1:2], func=AF.Exp, scale=-0.5),
                 reads=["sm3"], writes=["sm3"])
            S.op("dve", lambda h: h.scalar_tensor_tensor(out=N2[:], in0=ACC[:], scalar=sm3[:, 1:2], in1=cap("gFbc"),
                                                         op0=ALU.mult, op1=ALU.mult),
                 reads=[ACCk, "sm3", "cA"], writes=[N2k])
            S.dma("sp", lambda h: h.dma_start(out=y_d[r0:r0 + 128, :], in_=N2[:]), "sty", reads=[N2k],
                  writes=["ydram"])

        items = build_AB(0)
        run_items(items, 0, len(items))
        if NT > 1:
            enqueue_AB(1, 0)
        for tt in range(NT):
            if tt + 2 < NT:
                enqueue_AB(tt + 2, (tt + 1) * 128 - EARLY)
            emit_CD(tt)

        sp = S.E["sp"]
        for key, d in S.dsem.items():
            if key == "sty" or key.startswith("tap_"):
                sp.prog.append(lambda h, d=d: h.wait_ge(d[0], d[1]))
        S.emit()
    return nc, tap_d


def _host_layout(inp):
    f = np.float32
    w_in = np.asarray(inp["w_in"], f)[0]
    wq = np.asarray(inp["peer_wq"], f)[0]
    wall = np.empty((NWALL, 128, 4096), f)

    def blockify(w):
        kc = w.shape[0] // 128
        return w.reshape(kc, 128, w.shape[1]).transpose(1, 0, 2).reshape(128, kc * w.shape[1])

    for b, c0 in enumerate(WIN_COL0):
        wall[b] = blockify(w_in[:, c0:c0 + 512])
    for b in range(4):
        wall[10 + b] = blockify(wq[:, b * 512:(b + 1) * 512])
    wall[14] = blockify(np.asarray(inp["w_branch_a"], f)[0])
    wall[15] = blockify(np.asarray(inp["w_branch_b"], f)[0])
    wo = np.asarray(inp["w_out"], f)[0]
    wall[16] = blockify(wo[0:512])
    wall[17] = blockify(wo[512:1024])

    cA = np.zeros((128, NCA), f)

    def put(name, arr):
        a, b = CA[name]
        cA[:arr.shape[0], a:b] = arr

    tri = np.zeros((128, 128), f)
    ci = np.zeros((128, 2), f)
    for cp in range(128):
        ci[cp, cp // 64] = -1.0 / 16.0
        for c in range(128):
            if cp // 64 == c // 64 and cp > c:
                tri[cp, c] = -1.0 / 16.0
    put("trirev", tri)
    put("chunkind", ci)
    put("iota16", np.broadcast_to(np.arange(16, dtype=f)[None], (128, 16)))
    put("g1T", np.asarray(inp["norm1_g"], f)[0].reshape(8, 128).T)
    put("g2T", np.asarray(inp["norm2_g"], f)[0].reshape(8, 128).T)
    put("goutT", np.asarray(inp["gla_norm_g"], f)[0].reshape(4, 128).T)
    put("sbT", np.asarray(inp["sgu_b"], f)[0].T)
    put("lng", np.broadcast_to(np.asarray(inp["sgu_ln_g"], f)[0][None], (128, 512)))
    put("lnb", np.broadcast_to(np.asarray(inp["sgu_ln_b"], f)[0][None], (128, 512)))
    put("g2bc", np.broadcast_to(np.asarray(inp["norm2_g"], f)[0][None], (128, 1024)))
    put("gFbc", np.broadcast_to(np.asarray(inp["final_g"], f)[None], (128, 1024)))
    put("wga", np.concatenate([np.asarray(inp["w_gate_up"], f)[0], np.asarray(inp["b_gate"], f)], axis=0))

    cB = np.zeros((128, NCB), f)

    def putb(name, arr):
        a, b = CB[name]
        cB[:, a:b] = arr

    putb("ident", np.eye(128, dtype=f))
    putb("k1T", np.asarray(inp["peer_k1"], f)[0].T)
    putb("k2T", np.asarray(inp["peer_k2"], f)[0].T)
    putb("swT", np.asarray(inp["sgu_w"], f)[0].transpose(2, 0, 1).reshape(128, 512))
    pos = np.arange(128) // 64
    putb("mask", (pos[None, :] >= pos[:, None]).astype(f))
    putb("walr", blockify(w_in[:, 2048:2064]))
    return wall, cA, cB


_NC_CACHE = {}


def kernel(**inputs):
    x = np.ascontiguousarray(np.asarray(inputs["x"], np.float32))
    wall, cA, cB = _host_layout(inputs)
    puv = np.concatenate([np.asarray(inputs["peer_u"], np.float32)[0],
                          np.asarray(inputs["peer_v"], np.float32)[0]], axis=1)
    if "nc" not in _NC_CACHE:
        _NC_CACHE["nc"] = build_nc(SEQ // 128)[0]
    nc = _NC_CACHE["nc"]
    in_maps = [{"x": x[b], "wall": wall, "cA": cA, "cB": cB, "peer_uv": puv} for b in range(NCORES)]
    res = run_bass_kernel_spmd(nc, in_maps, core_ids=list(range(NCORES)))
    return np.stack([np.asarray(r["y"], np.float32) for r in res.results], axis=0)
```

```python
import numpy as np
from contextlib import ExitStack
from collections import deque
import concourse.bass as bass
import concourse.mybir as mybir
from concourse.bass_utils import run_bass_kernel_spmd

F32 = mybir.dt.float32
BF16 = mybir.dt.bfloat16
I32 = mybir.dt.int32
U32 = mybir.dt.uint32
AF = mybir.ActivationFunctionType
ALU = mybir.AluOpType
AX = mybir.AxisListType

D = 1024
SEQ = 4096
NCORES = 8
EPS = 1e-6
NEXP = 16384
NBLK = 14
NWALL = 18
WIN_COL0 = [0, 512, 1024, 1536, 2064, 2576, 3088, 3600, 4112, 4624]

CA = {}
_o = 0
for _n, _w in [("trirev", 128), ("chunkind", 2), ("iota16", 16), ("g1T", 8), ("g2T", 8), ("goutT", 4),
               ("sbT", 4), ("lng", 512), ("lnb", 512), ("g2bc", 1024), ("gFbc", 1024), ("wga", 512)]:
    CA[_n] = (_o, _o + _w)
    _o += _w
NCA = _o
CB = {}
_o = 0
for _n, _w in [("ident", 128), ("k1T", 128), ("k2T", 128), ("swT", 512), ("mask", 128), ("walr", 128)]:
    CB[_n] = (_o, _o + _w)
    _o += _w
NCB = _o


class Tok:
    __slots__ = ("sem", "key", "val", "eng")

    def __init__(self, sem, key, val, eng):
        self.sem, self.key, self.val, self.eng = sem, key, val, eng


class Eng:
    def __init__(self, name, sem):
        self.name, self.sem = name, sem
        self.n = 0
        self.seen = {}
        self.prog = []


class Sched:
    ENGS = ("pe", "act", "dve", "pool", "sp")

    def __init__(self, nc, stack):
        self.nc = nc
        self.stack = stack
        self.E = {}
        for n in self.ENGS:
            sem = stack.enter_context(nc.semaphore("e_" + n))
            self.E[n] = Eng(n, sem)
        self.dsem = {}
        self.lastw = {}
        self.readers = {}

    def _dsem(self, key):
        if key not in self.dsem:
            sem = self.stack.enter_context(self.nc.semaphore("d_" + key))
            self.dsem[key] = [sem, 0]
        return self.dsem[key]

    def _wait(self, eng, tok):
        if eng.seen.get(tok.key, 0) >= tok.val:
            return
        eng.seen[tok.key] = tok.val
        eng.prog.append(lambda h, s=tok.sem, v=tok.val: h.wait_ge(s, v))

    def op(self, engname, fn, reads=(), writes=()):
        eng = self.E[engname]
        for k in reads:
            w = self.lastw.get(k)
            if w is not None and not (w.eng is eng and engname == "pe"):
                self._wait(eng, w)
        for k in writes:
            w = self.lastw.get(k)
            if w is not None and not (w.eng is eng and engname == "pe"):
                self._wait(eng, w)
            for r in self.readers.get(k, {}).values():
                if not (r.eng is eng and engname == "pe"):
                    self._wait(eng, r)
        eng.n += 1
        tok = Tok(eng.sem, "e_" + engname, eng.n, eng)
        eng.prog.append(lambda h, f=fn, s=eng.sem: f(h).then_inc(s, 1))
        self._record(tok, reads, writes)
        return tok

    def dma(self, engname, fn, semkey, reads=(), writes=()):
        eng = self.E[engname]
        for k in reads:
            w = self.lastw.get(k)
            if w is not None:
                self._wait(eng, w)
        for k in writes:
            w = self.lastw.get(k)
            if w is not None:
                self._wait(eng, w)
            for r in self.readers.get(k, {}).values():
                self._wait(eng, r)
        d = self._dsem(semkey)
        d[1] += 16
        tok = Tok(d[0], "d_" + semkey, d[1], None)
        eng.prog.append(lambda h, f=fn, s=d[0]: f(h).then_inc(s, 16))
        self._record(tok, reads, writes)
        return tok

    def _record(self, tok, reads, writes):
        for k in reads:
            self.readers.setdefault(k, {})[tok.key] = tok
        for k in writes:
            self.lastw[k] = tok
            self.readers[k] = {}

    def emit(self):
        hmap = {"pe": "tensor", "act": "scalar", "dve": "vector", "pool": "gpsimd", "sp": "sync"}
        with self.nc.Block() as block:
            for n in self.ENGS:
                prog = self.E[n].prog

                def body(h, prog=prog):
                    for t in prog:
                        t(h)

                getattr(block, hmap[n])(body)


class Defer:
    def __init__(self):
        self.items = []

    def op(self, *a, **k):
        self.items.append(("op", a, k))

    def dma(self, *a, **k):
        self.items.append(("dma", a, k))


def build_nc(NT=32, taps=None):
    taps = taps or []
    nc = bass.Bass("TRN2", target_bir_lowering=False)
    x_d = nc.dram_tensor("x", [SEQ, D], F32, kind="ExternalInput").ap()
    wall_d = nc.dram_tensor("wall", [NWALL, 128, 4096], F32, kind="ExternalInput").ap()
    cA_d = nc.dram_tensor("cA", [128, NCA], F32, kind="ExternalInput").ap()
    cB_d = nc.dram_tensor("cB", [128, NCB], F32, kind="ExternalInput").ap()
    puv_d = nc.dram_tensor("peer_uv", [NEXP, 2 * D], F32, kind="ExternalInput").ap()
    y_d = nc.dram_tensor("y", [SEQ, D], F32, kind="ExternalOutput").ap()
    wsc_d = nc.dram_tensor("wsc", [NBLK, 128, 4096], BF16, kind="Internal").ap()
    tbl_d = nc.dram_tensor("tbl", [NEXP, 2 * D], BF16, kind="Internal").ap()
    tap_d = {}

    with ExitStack() as st:
        st.enter_context(nc.allow_low_precision("bf16 matmul operands, fp32 accumulation"))
        S = Sched(nc, st)

        sb_total = [0]

        def sb(name, shape, dt):
            n = 1
            for d_ in shape[1:]:
                n *= d_
            sb_total[0] += n * (2 if dt == BF16 else 4)
            return st.enter_context(nc.sbuf_tensor(name, shape, dt))

        def ps(name, shape, dt):
            return st.enter_context(nc.psum_tensor(name, shape, dt))

        cA = sb("cA_sb", [128, NCA], F32)
        ident = sb("ident", [128, 128], BF16)
        k1T = sb("k1T", [128, 128], BF16)
        k2T = sb("k2T", [128, 128], BF16)
        swT = sb("swT", [128, 4, 128], BF16)
        walr = sb("walr", [128, 8, 16], BF16)
        wa = sb("wa", [128, 4, 1024], BF16)
        wb = sb("wb", [128, 4, 1024], BF16)
        wo = sb("wo", [128, 8, 1024], BF16)
        NWB = 2
        wbuf = [sb(f"wbuf{i}", [128, 8, 512], BF16) for i in range(NWB)]
        NR = 16
        ring = sb("ring", [128, NR * 2048], BF16)
        NDG = 4
        dg = sb("dg", [128, NDG, 128], BF16)
        acc = [sb(f"acc{i}", [128, 1024], F32) for i in range(2)]
        state = sb("state", [128, 512], F32)
        state_bf = [sb(f"state_bf{i}", [128, 512], BF16) for i in range(2)]
        junk_a = sb("junk_a", [128, 1024], BF16)
        jd_raw = sb("jd_raw", [128, 512], F32)
        junk_d = jd_raw[:].bitcast(BF16)
        s_wk_t = sb("s_wk", [128, 256], F32)
        s_wk = s_wk_t[:]
        xn = sb("xn", [128, 1024], BF16)
        nT = sb("nT", [128, 8, 128], BF16)
        qT = sb("qT", [128, 512], BF16)
        k_sb = sb("k_sb", [128, 512], F32)
        v_bf = sb("v_bf", [128, 512], BF16)
        silu_r = sb("silu_r", [128, 512], F32)
        sugv = sb("sugv", [128, 1024], F32)
        sga = sb("sga", [128, 1024], F32)
        sgb = sb("sgb", [128, 1024], F32)
        xs = sga
        xs2 = sugv
        alT = sb("alT", [32, 128], F32)
        Lg = sb("Lg", [128, 512], F32)
        Ek = sb("Ek", [128, 512], F32)
        dec = sb("dec", [128, 8], F32)
        kdec = sb("kdec", [128, 512], BF16)
        ya = Lg
        ya_bf = sb("ya_bf", [128, 512], BF16)
        yaT = sb("yaT", [128, 4, 128], BF16)
        vn = Ek
        vn_bf = sb("vn_bf", [128, 512], BF16)
        yb = sb("yb", [128, 512], BF16)
        ybT = sb("ybT", [128, 4, 128], BF16)
        mgq = sb("mgq", [128, 2048], BF16)
        mg_bf = mgq[:, 0:1024]
        mgT = mgq[:, 1024:2048].rearrange("p (c t) -> p c t", c=8)
        qpT = mgq[:].rearrange("p (g t) -> p g t", g=16)
        n2 = [sb(f"n2_{i}", [128, 1024], F32) for i in range(2)]
        s_sb = sb("s_sb", [128, 16, 128], F32)
        V16 = sb("V16", [128, 16, 16], F32)
        I16 = sb("I16", [128, 16, 16], U32)
        VAL = sb("VAL", [128, 8, 16], F32)
        CI = sb("CI", [128, 8, 16], U32)
        IA = sb("IA", [128, 8, 16], U32)
        IB = sb("IB", [128, 8, 16], U32)
        IAf = IA[:].bitcast(F32)
        IBf = IB[:].bitcast(F32)
        E1 = s_wk_t[:, 0:128].rearrange("p (h k) -> p h k", h=8)
        E2 = s_wk_t[:, 128:256].rearrange("p (h k) -> p h k", h=8)
        idxf = sb("idxf", [128, 128], F32)
        idx = [sb(f"idx{i}", [128, 128], I32) for i in range(2)]
        ge = sb("ge", [128, 8, 16], F32)
        gs = sb("gs", [128, 8], F32)
        gates = [sb(f"gates{i}", [128, 128], F32) for i in range(2)]
        hh = sb("hh", [128, 128], F32)
        aw = sb("aw", [128, 128], F32)
        sm1 = sb("sm1", [128, 2], F32)
        smo = sb("smo", [128, 8], F32)
        smln = sb("smln", [128, 1], F32)
        sm2 = sb("sm2", [128, 2], F32)
        sm3 = sb("sm3", [128, 2], F32)
        ghalf = sb("ghalf", [128, 4], F32)
        bnst = sb("bnst", [128, 6], F32)
        bnag = sb("bnag", [128, 2], F32)

        tp = ps("tp", [128, 1024], BF16)
        pacc = [ps(f"pacc{i}", [128, 512], F32) for i in range(2)]
        NPB = 5
        pbank = [ps(f"pb{i}", [128, 512], F32) for i in range(NPB)]
        pctr = [0]

        def bank():
            i = pctr[0] % NPB
            pctr[0] += 1
            return pbank[i], f"pb{i}"

        def cap(name):
            a, b = CA[name]
            return cA[:, a:b]

        S.dma("sp", lambda h: h.dma_start(out=cA[:], in_=cA_d), "ldc", writes=["cA"])
        stg = ring[:].bitcast(F32)
        S.dma("sp", lambda h: h.dma_start(out=stg[:, 0:NCB], in_=cB_d), "ldc2", writes=["stg0"])

        def cbp(name):
            a, b = CB[name]
            return stg[:, a:b]

        S.op("dve", lambda h: h.memset(alT[:], 1.0), writes=["alT"])
        S.op("dve", lambda h: h.tensor_scalar(out=ghalf[:], in0=cap("goutT"), scalar1=0.5, scalar2=None, op0=ALU.mult),
             reads=["cA"], writes=["ghalf"])
        S.op("dve", lambda h: h.memset(state[:], 0.0), writes=["state"])
        S.op("dve", lambda h: h.tensor_copy(out=ident[:], in_=cbp("ident")), reads=["stg0"], writes=["ident"])
        S.op("dve", lambda h: h.tensor_copy(out=k1T[:], in_=cbp("k1T")), reads=["stg0"], writes=["k1T"])
        S.op("dve", lambda h: h.tensor_copy(out=k2T[:], in_=cbp("k2T")), reads=["stg0"], writes=["k2T"])
        S.op("dve", lambda h: h.tensor_copy(out=walr[:].rearrange("p c n -> p (c n)"), in_=cbp("walr")),
             reads=["stg0"], writes=["walr"])
        msk = cbp("mask")
        S.op("dve", lambda h: h.tensor_tensor(
            out=swT[:], in0=cbp("swT").rearrange("p (g i) -> p g i", g=4),
            in1=msk.unsqueeze(1).to_broadcast([128, 4, 128]), op=ALU.mult),
            reads=["stg0"], writes=["swT"])
        for b in range(NBLK):
            S.dma("pool", lambda h, b=b: h.dma_start(out=wsc_d[b], in_=wall_d[b]), f"wcv{b}", writes=[f"wsc{b}"])
        S.dma("pool", lambda h: h.dma_start(out=wa[:].rearrange("p c n -> p (c n)"), in_=wall_d[14]), "wcva", writes=["wa"])
        S.dma("pool", lambda h: h.dma_start(out=wb[:].rearrange("p c n -> p (c n)"), in_=wall_d[15]), "wcvb", writes=["wb"])
        for hb in range(2):
            S.dma("pool", lambda h, hb=hb: h.dma_start(
                out=wo[:, hb * 4:hb * 4 + 4, :].rearrange("p c n -> p (c n)"), in_=wall_d[16 + hb]),
                f"wcvo{hb}", writes=["wo"])
        NCH = 32
        RCH = NEXP // NCH
        TBL_KEYS = [f"tbl{i}" for i in range(NCH)]
        for i in range(NCH):
            S.dma("pool", lambda h, i=i: h.dma_start(out=tbl_d[i * RCH:(i + 1) * RCH, :],
                                                     in_=puv_d[i * RCH:(i + 1) * RCH, :]),
                  "tcv", writes=[TBL_KEYS[i]])

        wctr = [0]
        rctr = [0]
        dctr = [0]
        ring_alias = {b: [f"stg{min(b * 2048 * 2 // (4096 * 4), 1)}"] for b in range(NR)}

        def build_AB(tt):
            Q = Defer()
            p = tt % 2
            ACC, ACCk = acc[p], f"acc{p}"
            N2, N2k = n2[p], f"n2_{p}"
            IDX, IDXk = idx[p], f"idx{p}"
            GT, GTk = gates[p], f"gates{p}"
            r0 = tt * 128
            T0 = (tt == 0)

            def tap(name, ap, key, shape):
                if name not in taps or not T0:
                    return
                t = nc.dram_tensor("tap_" + name, list(shape), ap.dtype, kind="ExternalOutput").ap()
                tap_d[name] = t
                Q.dma("sp", lambda h: h.dma_start(out=t, in_=ap), "tap_" + name, reads=[key])

            def rstd_from_ss(ss_ap, out_ap, n, key):
                Q.op("act", lambda h: h.activation(out=out_ap, in_=ss_ap, func=AF.Ln, scale=1.0 / n, bias=EPS),
                     reads=[key], writes=[key])
                Q.op("act", lambda h: h.activation(out=out_ap, in_=out_ap, func=AF.Exp, scale=-0.5),
                     reads=[key], writes=[key])

            def transposes(src_tile, nchunks, srckey):
                for c in range(nchunks):
                    Q.op("pe", lambda h, c=c: h.transpose(out=tp[:, c * 128:(c + 1) * 128],
                                                         in_=src_tile[:, c * 128:(c + 1) * 128], identity=ident[:]),
                         reads=[srckey, "ident"], writes=["tp"])

            def wload(blk):
                i = wctr[0] % NWB
                wctr[0] += 1
                Q.dma("sp", lambda h, i=i, blk=blk: h.dma_start(out=wbuf[i][:].rearrange("p c n -> p (c n)"),
                                                                in_=wsc_d[blk]),
                      f"wl{i}", reads=[f"wsc{blk}"], writes=[f"wbuf{i}"])
                return wbuf[i], f"wbuf{i}"

            if tt == 0:
                Q.dma("sp", lambda h: h.dma_start(out=xs[:], in_=x_d[0:128, :]), "ldx", writes=["sga"])
            Q.op("act", lambda h: h.activation(out=junk_a[:], in_=xs[:], func=AF.Square, accum_out=sm1[:, 0:1]),
                 reads=["sga"], writes=["junk_a", "sm1"])
            rstd_from_ss(sm1[:, 0:1], sm1[:, 1:2], D, "sm1")
            Q.op("act", lambda h: h.activation(out=xn[:], in_=xs[:], func=AF.Copy, scale=sm1[:, 1:2]),
                 reads=["sga", "sm1"], writes=["xn"])
            transposes(xn, 8, "xn")
            Q.op("dve", lambda h: h.tensor_tensor(out=nT[:], in0=tp[:].rearrange("p (c t) -> p c t", c=8),
                                                  in1=cap("g1T").unsqueeze(2).to_broadcast([128, 8, 128]),
                                                  op=ALU.mult),
                 reads=["tp", "cA"], writes=["nT"])

            w, wk = wload(0)
            pq, pqk = bank()
            for hd in range(4):
                for c in range(8):
                    Q.op("pe", lambda h, hd=hd, c=c, w=w, pq=pq: h.matmul(
                        out=pq[:, hd * 128:(hd + 1) * 128], lhsT=w[:, c, hd * 128:(hd + 1) * 128],
                        rhs=nT[:, c, :], start=(c == 0), stop=(c == 7)),
                        reads=[wk, "nT"], writes=[pqk])
            Q.op("act", lambda h, pq=pq: h.activation(out=qT[:], in_=pq[:], func=AF.Copy, scale=128.0 ** -0.5),
                 reads=[pqk], writes=["qT"])

            def proj_block(blk):
                w, wk = wload(blk)
                pb, pbk = bank()
                for c in range(8):
                    Q.op("pe", lambda h, c=c, w=w, pb=pb: h.matmul(out=pb[:], lhsT=nT[:, c, :], rhs=w[:, c, :],
                                                                  start=(c == 0), stop=(c == 7)),
                         reads=[wk, "nT"], writes=[pbk])
                return pb, pbk

            pa, pak = bank()
            for c in range(8):
                Q.op("pe", lambda h, c=c, pa=pa: h.matmul(out=pa[0:16, 0:128], lhsT=walr[:, c, :], rhs=nT[:, c, :],
                                                         start=(c == 0), stop=(c == 7)),
                     reads=["walr", "nT"], writes=[pak])
            Q.op("act", lambda h, pa=pa: h.activation(out=alT[0:16, :], in_=pa[0:16, 0:128], func=AF.Copy),
                 reads=[pak], writes=["alT"])
            pz, pzk = bank()
            Q.op("pe", lambda h, pz=pz: h.matmul(out=pz[:], lhsT=alT[0:17, :], rhs=cap("wga")[0:17, :],
                                                 start=True, stop=True),
                 reads=["alT", "cA"], writes=[pzk])
            Q.op("act", lambda h, pz=pz: h.activation(out=Lg[:], in_=pz[:], func=AF.Exp, scale=-1.0),
                 reads=[pzk], writes=["Lg"])
            Q.op("act", lambda h: h.activation(out=Lg[:], in_=Lg[:], func=AF.Ln, bias=1.0, scale=1.0),
                 reads=["Lg"], writes=["Lg"])
            pb, pbk = proj_block(1)
            Q.op("act", lambda h, pb=pb: h.activation(out=k_sb[:], in_=pb[:], func=AF.Copy), reads=[pbk], writes=["k_sb"])
            pb, pbk = proj_block(2)
            Q.op("act", lambda h, pb=pb: h.activation(out=v_bf[:], in_=pb[:], func=AF.Copy), reads=[pbk], writes=["v_bf"])
            prv, prvk = bank()
            Q.op("pe", lambda h, prv=prv: h.matmul(out=prv[:], lhsT=cap("trirev"), rhs=Lg[:], start=True, stop=True),
                 reads=["cA", "Lg"], writes=[prvk])
            ptt, pttk = bank()
            for hd in range(4):
                Q.op("pe", lambda h, hd=hd, ptt=ptt: h.matmul(out=ptt[:, hd * 2:hd * 2 + 2],
                                                              lhsT=Lg[:, hd * 128:(hd + 1) * 128],
                                                              rhs=cap("chunkind"), start=True, stop=True),
                     reads=["cA", "Lg"], writes=[pttk])
            Q.op("act", lambda h, prv=prv: h.activation(out=Ek[:], in_=prv[:], func=AF.Exp), reads=[prvk], writes=["Ek"])
            Q.op("act", lambda h, ptt=ptt: h.activation(out=dec[:], in_=ptt[:, 0:8], func=AF.Exp), reads=[pttk], writes=["dec"])
            Q.op("dve", lambda h: h.tensor_tensor(out=kdec[:], in0=k_sb[:], in1=Ek[:], op=ALU.mult),
                 reads=["k_sb", "Ek"], writes=["kdec"])
            pb, pbk = proj_block(3)
            Q.op("act", lambda h, pb=pb: h.activation(out=silu_r[:], in_=pb[:], func=AF.Tanh, scale=0.5), reads=[pbk], writes=["silu_r"])
            Q.op("dve", lambda h, pb=pb: h.scalar_tensor_tensor(out=silu_r[:], in0=silu_r[:], scalar=1.0, in1=pb[:],
                                                                op0=ALU.add, op1=ALU.mult),
                 reads=[pbk, "silu_r"], writes=["silu_r"])
            for i, blk in enumerate((4, 5)):
                pb, pbk = proj_block(blk)
                Q.op("act", lambda h, pb=pb, i=i: h.activation(out=sugv[:, i * 512:(i + 1) * 512], in_=pb[:],
                                                               func=AF.Gelu_apprx_tanh),
                     reads=[pbk], writes=["sugv"])
            for i, blk in enumerate((6, 7)):
                pb, pbk = proj_block(blk)
                Q.op("act", lambda h, pb=pb, i=i: h.activation(out=sga[:, i * 512:(i + 1) * 512], in_=pb[:], func=AF.Tanh, scale=0.5),
                     reads=[pbk], writes=["sga"])
            for i, blk in enumerate((8, 9)):
                pb, pbk = proj_block(blk)
                Q.op("act", lambda h, pb=pb, i=i: h.activation(out=sgb[:, i * 512:(i + 1) * 512], in_=pb[:], func=AF.Tanh, scale=0.5),
                     reads=[pbk], writes=["sgb"])

            po, pok = bank()
            for j in range(2):
                pkv, pkvk = bank()
                for hd in range(4):
                    Q.op("pe", lambda h, hd=hd, j=j, pkv=pkv: h.matmul(
                        out=pkv[:, hd * 128:(hd + 1) * 128],
                        lhsT=kdec[64 * j:64 * j + 64, hd * 128:(hd + 1) * 128],
                        rhs=v_bf[64 * j:64 * j + 64, hd * 128:(hd + 1) * 128], start=True, stop=True),
                        reads=["kdec", "v_bf"], writes=[pkvk])
                for hd in range(4):
                    Q.op("dve", lambda h, hd=hd, j=j, pkv=pkv: h.scalar_tensor_tensor(
                        out=state[:, hd * 128:(hd + 1) * 128], in0=state[:, hd * 128:(hd + 1) * 128],
                        scalar=dec[:, hd * 2 + j:hd * 2 + j + 1], in1=pkv[:, hd * 128:(hd + 1) * 128],
                        op0=ALU.mult, op1=ALU.add),
                        reads=[f"state{hd}", "dec", pkvk], writes=[f"state{hd}"])
                Q.op("act", lambda h, j=j: h.activation(out=state_bf[j][:], in_=state[:], func=AF.Copy),
                     reads=[f"state{hd}" for hd in range(4)], writes=[f"state_bf{j}"])
                for hd in range(4):
                    Q.op("pe", lambda h, hd=hd, j=j, po=po: h.matmul(
                        out=po[64 * j:64 * j + 64, hd * 128:(hd + 1) * 128],
                        lhsT=qT[:, hd * 128 + 64 * j:hd * 128 + 64 * j + 64],
                        rhs=state_bf[j][:, hd * 128:(hd + 1) * 128], start=True, stop=True),
                        reads=["qT", f"state_bf{j}"], writes=[pok])
            for hd in range(4):
                Q.op("act", lambda h, hd=hd, po=po: h.activation(out=junk_a[:, hd * 128:(hd + 1) * 128],
                                                                 in_=po[:, hd * 128:(hd + 1) * 128], func=AF.Square,
                                                                 accum_out=smo[:, hd:hd + 1]),
                     reads=[pok], writes=["junk_a", "smo"])
            rstd_from_ss(smo[:, 0:4], smo[:, 4:8], 128, "smo")
            Q.op("dve", lambda h, po=po: h.tensor_tensor(out=ya[:].rearrange("p (g v) -> p g v", g=4),
                                                         in0=po[:].rearrange("p (g v) -> p g v", g=4),
                                                         in1=smo[:, 4:8].unsqueeze(2).to_broadcast([128, 4, 128]),
                                                         op=ALU.mult),
                 reads=[pok, "smo"], writes=["Lg"])
            Q.op("dve", lambda h: h.tensor_tensor(out=ya_bf[:], in0=ya[:], in1=silu_r[:], op=ALU.mult),
                 reads=["Lg", "silu_r"], writes=["ya_bf"])
            transposes(ya_bf, 4, "ya_bf")
            Q.op("dve", lambda h: h.tensor_tensor(out=yaT[:], in0=tp[:, 0:512].rearrange("p (c t) -> p c t", c=4),
                                                  in1=ghalf[:].unsqueeze(2).to_broadcast([128, 4, 128]),
                                                  op=ALU.mult),
                 reads=["tp", "ghalf"], writes=["yaT"])

            svg = sugv[:, 512:1024]
            Q.op("dve", lambda h: h.bn_stats(out=bnst[:], in_=svg), reads=["sugv"], writes=["bnst"])
            Q.op("dve", lambda h: h.bn_aggr(out=bnag[:], in_=bnst[:]), reads=["bnst"], writes=["bnag"])
            Q.op("act", lambda h: h.activation(out=smln[:], in_=bnag[:, 1:2], func=AF.Ln, scale=1.0, bias=EPS),
                 reads=["bnag"], writes=["smln"])
            Q.op("act", lambda h: h.activation(out=smln[:], in_=smln[:], func=AF.Exp, scale=-0.5),
                 reads=["smln"], writes=["smln"])
            Q.op("dve", lambda h: h.tensor_scalar(out=vn[:], in0=svg, scalar1=bnag[:, 0:1], scalar2=smln[:, 0:1],
                                                  op0=ALU.subtract, op1=ALU.mult),
                 reads=["sugv", "bnag", "smln"], writes=["Ek"])
            Q.op("dve", lambda h: h.tensor_tensor(out=vn[:], in0=vn[:], in1=cap("lng"), op=ALU.mult),
                 reads=["Ek", "cA"], writes=["Ek"])
            Q.op("dve", lambda h: h.tensor_tensor(out=vn_bf[:], in0=vn[:], in1=cap("lnb"), op=ALU.add),
                 reads=["Ek", "cA"], writes=["vn_bf"])
            pm, pmk = bank()
            for g in range(4):
                Q.op("pe", lambda h, g=g, pm=pm: h.matmul(out=pm[:, g * 128:(g + 1) * 128], lhsT=swT[:, g, :],
                                                         rhs=vn_bf[:, g * 128:(g + 1) * 128], start=True, stop=True),
                     reads=["swT", "vn_bf"], writes=[pmk])
            for g in range(4):
                Q.op("dve", lambda h, g=g, pm=pm: h.scalar_tensor_tensor(
                    out=yb[:, g * 128:(g + 1) * 128], in0=pm[:, g * 128:(g + 1) * 128],
                    scalar=cap("sbT")[:, g:g + 1], in1=sugv[:, g * 128:(g + 1) * 128], op0=ALU.add, op1=ALU.mult),
                    reads=[pmk, "cA", "sugv"], writes=["yb"])
            transposes(yb, 4, "yb")
            Q.dma("sp", lambda h: h.dma_start(out=xs2[:], in_=x_d[r0:r0 + 128, :]), "ldx2", writes=["sugv"])
            Q.op("act", lambda h: h.activation(out=ybT[:].rearrange("p c t -> p (c t)"), in_=tp[:, 0:512], func=AF.Copy),
                 reads=["tp"], writes=["ybT"])

            for half in range(2):
                pA, pAk = bank()
                for c in range(4):
                    Q.op("pe", lambda h, c=c, half=half, pA=pA: h.matmul(
                        out=pA[:], lhsT=yaT[:, c, :], rhs=wa[:, c, half * 512:(half + 1) * 512],
                        start=(c == 0), stop=(c == 3)), reads=["yaT", "wa"], writes=[pAk])
                Q.op("dve", lambda h, half=half, pA=pA: h.scalar_tensor_tensor(
                    out=sga[:, half * 512:(half + 1) * 512], in0=sga[:, half * 512:(half + 1) * 512], scalar=1.0,
                    in1=pA[:], op0=ALU.add, op1=ALU.mult), reads=[pAk, "sga"], writes=["sga"])
                pB, pBk = bank()
                for c in range(4):
                    Q.op("pe", lambda h, c=c, half=half, pB=pB: h.matmul(
                        out=pB[:], lhsT=ybT[:, c, :], rhs=wb[:, c, half * 512:(half + 1) * 512],
                        start=(c == 0), stop=(c == 3)), reads=["ybT", "wb"], writes=[pBk])
                Q.op("dve", lambda h, half=half, pB=pB: h.scalar_tensor_tensor(
                    out=sgb[:, half * 512:(half + 1) * 512], in0=sgb[:, half * 512:(half + 1) * 512], scalar=1.0,
                    in1=pB[:], op0=ALU.add, op1=ALU.mult), reads=[pBk, "sgb"], writes=["sgb"])
            Q.op("dve", lambda h: h.tensor_tensor(out=mg_bf, in0=sga[:], in1=sgb[:], op=ALU.add),
                 reads=["sga", "sgb"], writes=["mgq"])
            transposes(mg_bf, 8, "mgq")
            Q.op("act", lambda h: h.activation(out=mgq[:, 1024:2048], in_=tp[:], func=AF.Copy, scale=0.5),
                 reads=["tp"], writes=["mgq"])
            for half in range(2):
                pD, pDk = bank()
                for c in range(8):
                    Q.op("pe", lambda h, c=c, half=half, pD=pD: h.matmul(
                        out=pD[:], lhsT=mgT[:, c, :], rhs=wo[:, c, half * 512:(half + 1) * 512],
                        start=(c == 0), stop=(c == 7)), reads=["mgq", "wo"], writes=[pDk])
                Q.op("dve", lambda h, half=half, pD=pD: h.tensor_tensor(
                    out=ACC[:, half * 512:(half + 1) * 512], in0=pD[:], in1=xs2[:, half * 512:(half + 1) * 512],
                    op=ALU.add), reads=[pDk, "sugv"], writes=[ACCk])
            tap("h", ACC[:], ACCk, [128, 1024])
            if tt + 1 < NT:
                Q.dma("sp", lambda h: h.dma_start(out=xs[:], in_=x_d[r0 + 128:r0 + 256, :]), "ldx", writes=["sga"])

            Q.op("act", lambda h: h.activation(out=junk_a[:], in_=ACC[:], func=AF.Square, accum_out=sm2[:, 0:1]),
                 reads=[ACCk], writes=["junk_a", "sm2"])
            rstd_from_ss(sm2[:, 0:1], sm2[:, 1:2], D, "sm2")
            Q.op("dve", lambda h: h.scalar_tensor_tensor(out=N2[:], in0=ACC[:], scalar=sm2[:, 1:2], in1=cap("g2bc"),
                                                         op0=ALU.mult, op1=ALU.mult),
                 reads=[ACCk, "sm2", "cA"], writes=[N2k])
            Q.op("act", lambda h: h.activation(out=xn[:], in_=N2[:], func=AF.Copy), reads=[N2k], writes=["xn"])
            transposes(xn, 8, "xn")
            Q.op("act", lambda h: h.activation(out=nT[:].rearrange("p c t -> p (c t)"), in_=tp[:], func=AF.Copy),
                 reads=["tp"], writes=["nT"])

            for blk in range(4):
                w, wk = wload(10 + blk)
                pb, pbk = bank()
                for gg in range(4):
                    for c in range(8):
                        Q.op("pe", lambda h, gg=gg, c=c, w=w, pb=pb: h.matmul(
                            out=pb[:, gg * 128:(gg + 1) * 128], lhsT=w[:, c, gg * 128:(gg + 1) * 128],
                            rhs=nT[:, c, :], start=(c == 0), stop=(c == 7)),
                            reads=[wk, "nT"], writes=[pbk])
                Q.op("act", lambda h, blk=blk, pb=pb: h.activation(
                    out=qpT[:, blk * 4:(blk + 1) * 4, :].rearrange("p g t -> p (g t)"), in_=pb[:], func=AF.Copy),
                    reads=[pbk], writes=["mgq"])
            for blk in range(4):
                pb, pbk = bank()
                for gg in range(4):
                    g = blk * 4 + gg
                    kk, kkk = (k1T, "k1T") if g % 2 == 0 else (k2T, "k2T")
                    Q.op("pe", lambda h, g=g, gg=gg, kk=kk, pb=pb: h.matmul(
                        out=pb[:, gg * 128:(gg + 1) * 128], lhsT=qpT[:, g, :], rhs=kk[:], start=True, stop=True),
                        reads=["mgq", kkk], writes=[pbk])
                Q.op("act", lambda h, blk=blk, pb=pb: h.activation(
                    out=s_sb[:, blk * 4:(blk + 1) * 4, :].rearrange("p g t -> p (g t)"), in_=pb[:], func=AF.Copy),
                    reads=[pbk], writes=["s_sb"])

            for g in range(16):
                sg = s_sb[:, g, :]
                Q.op("dve", lambda h, g=g, sg=sg: h.max(out=V16[:, g, 0:8], in_=sg), reads=["s_sb"], writes=["V16"])
                Q.op("dve", lambda h, g=g, sg=sg: h.max_index(out=I16[:, g, 0:8], in_max=V16[:, g, 0:8], in_values=sg),
                     reads=["s_sb", "V16"], writes=["I16"])
                Q.op("dve", lambda h, g=g, sg=sg: h.match_replace(out=s_wk[:, 0:128], in_to_replace=V16[:, g, 0:8],
                                                                  in_values=sg, imm_value=-1e30),
                     reads=["s_sb", "V16"], writes=["s_wk"])
                Q.op("dve", lambda h, g=g: h.max(out=V16[:, g, 8:16], in_=s_wk[:, 0:128]), reads=["s_wk"], writes=["V16"])
                Q.op("dve", lambda h, g=g: h.max_index(out=I16[:, g, 8:16], in_max=V16[:, g, 8:16],
                                                       in_values=s_wk[:, 0:128]),
                     reads=["s_wk", "V16"], writes=["I16"])
            Q.op("dve", lambda h: h.tensor_copy(out=I16[:].bitcast(F32), in_=I16[:]), reads=["I16"], writes=["I16"])
            V16v = V16[:].rearrange("p (h t) r -> p h t r", t=2)
            I16v = I16[:].bitcast(F32).rearrange("p (h t) r -> p h t r", t=2)
            grid = s_sb[:].rearrange("p g n -> p (g n)").rearrange("p (h a b) -> p h a b", h=8, a=16)
            Q.op("dve", lambda h: h.tensor_tensor(out=grid, in0=V16v[:, :, 0, :].unsqueeze(3).to_broadcast([128, 8, 16, 16]),
                                                  in1=V16v[:, :, 1, :].unsqueeze(2).to_broadcast([128, 8, 16, 16]),
                                                  op=ALU.add),
                 reads=["V16", "s_sb"], writes=["s_sb"])
            candf = s_sb[:].rearrange("p g n -> p (g n)").rearrange("p (h c) -> p h c", h=8)
            for hd in range(8):
                ch = candf[:, hd, :]
                Q.op("dve", lambda h, hd=hd, ch=ch: h.max(out=VAL[:, hd, 0:8], in_=ch), reads=["s_sb"], writes=["VAL"])
                Q.op("dve", lambda h, hd=hd, ch=ch: h.max_index(out=CI[:, hd, 0:8], in_max=VAL[:, hd, 0:8], in_values=ch),
                     reads=["s_sb", "VAL"], writes=["CI"])
                Q.op("dve", lambda h, hd=hd, ch=ch: h.match_replace(out=s_wk, in_to_replace=VAL[:, hd, 0:8],
                                                                    in_values=ch, imm_value=-1e30),
                     reads=["s_sb", "VAL"], writes=["s_wk"])
                Q.op("dve", lambda h, hd=hd: h.max(out=VAL[:, hd, 8:16], in_=s_wk), reads=["s_wk"], writes=["VAL"])
                Q.op("dve", lambda h, hd=hd: h.max_index(out=CI[:, hd, 8:16], in_max=VAL[:, hd, 8:16], in_values=s_wk),
                     reads=["s_wk", "VAL"], writes=["CI"])
            Q.op("dve", lambda h: h.tensor_single_scalar(out=IA[:], in_=CI[:], scalar=4, op=ALU.logical_shift_right),
                 reads=["CI"], writes=["IA"])
            Q.op("dve", lambda h: h.tensor_single_scalar(out=IB[:], in_=CI[:], scalar=15, op=ALU.bitwise_and),
                 reads=["CI"], writes=["IB"])
            Q.op("dve", lambda h: h.tensor_copy(out=IAf, in_=IA[:]), reads=["IA"], writes=["IA"])
            Q.op("dve", lambda h: h.tensor_copy(out=IBf, in_=IB[:]), reads=["IB"], writes=["IB"])
            iob = cap("iota16").unsqueeze(1).unsqueeze(1).to_broadcast([128, 8, 16, 16])
            for (If, Ifk, half, Eo, Ek_) in ((IAf, "IA", 0, E1, "s_wk"), (IBf, "IB", 1, E2, "s_wk")):
                Q.op("dve", lambda h, If=If: h.tensor_tensor(out=grid, in0=If.unsqueeze(3).to_broadcast([128, 8, 16, 16]),
                                                             in1=iob, op=ALU.is_equal),
                     reads=[Ifk, "cA"], writes=["s_sb"])
                Q.op("dve", lambda h, half=half: h.tensor_tensor(
                    out=grid, in0=grid, in1=I16v[:, :, half, :].unsqueeze(2).to_broadcast([128, 8, 16, 16]), op=ALU.mult),
                    reads=["s_sb", "I16"], writes=["s_sb"])
                Q.op("dve", lambda h, Eo=Eo: h.tensor_reduce(out=Eo, in_=grid, axis=AX.X, op=ALU.add),
                     reads=["s_sb"], writes=[Ek_])
            Q.op("dve", lambda h: h.scalar_tensor_tensor(out=idxf[:], in0=s_wk_t[:, 0:128], scalar=128.0,
                                                         in1=s_wk_t[:, 128:256], op0=ALU.mult, op1=ALU.add),
                 reads=["s_wk"], writes=["idxf"])
            Q.op("dve", lambda h: h.tensor_copy(out=IDX[:], in_=idxf[:]), reads=["idxf"], writes=[IDXk])
            Q.op("dve", lambda h: h.tensor_tensor(out=ge[:], in0=VAL[:], in1=VAL[:, :, 0:1].to_broadcast([128, 8, 16]),
                                                  op=ALU.subtract), reads=["VAL"], writes=["ge"])
            Q.op("act", lambda h: h.activation(out=ge[:], in_=ge[:], func=AF.Exp), reads=["ge"], writes=["ge"])
            Q.op("dve", lambda h: h.tensor_reduce(out=gs[:], in_=ge[:], axis=AX.X, op=ALU.add), reads=["ge"], writes=["gs"])
            Q.op("dve", lambda h: h.reciprocal(out=gs[:], in_=gs[:]), reads=["gs"], writes=["gs"])
            Q.op("dve", lambda h: h.tensor_tensor(out=GT[:].rearrange("p (h k) -> p h k", h=8), in0=ge[:],
                                                  in1=gs[:].unsqueeze(2).to_broadcast([128, 8, 16]), op=ALU.mult),
                 reads=["ge", "gs"], writes=[GTk])
            tap("idxf", idxf[:], "idxf", [128, 128])
            tap("gates", GT[:], GTk, [128, 128])
            return Q.items

        def run_items(items, lo, hi):
            for kind, a, k in items[lo:hi]:
                getattr(S, kind)(*a, **k)

        GRP = 2
        X_LAT = 1.0
        PE_LAT = 2.0
        DMA_LAT = 5.0
        BUDGET = 3
        EARLY = 100
        TAIL_DELAY = 8.0
        TOPK_KEYS = {"s_sb", "V16", "I16", "s_wk", "I16f", "VAL", "CI", "IA", "IB", "IAf", "IBf", "E1", "E2",
                     "idxf", "ge", "gs"}

        pending = deque()
        G = [0]
        floor_t = [0.0]

        def lat_from(eng):
            return {"dma": DMA_LAT, "pe": PE_LAT}.get(eng, X_LAT)

        tails = deque()
        gcnt = {}

        def enqueue_AB(t, base):
            items = build_AB(t)
            i_tail = len(items)
            for i, it in enumerate(items):
                if it[0] == "op" and it[1][0] == "dve" and "V16" in it[2].get("writes", ()):
                    i_tail = i
                    break
            acck = f"acc{t % 2}"
            barrier = float((t - 1) * 128)
            lw = {}
            lr = {}
            times = []
            t0 = max(float(base), floor_t[0])
            for it in items:
                eng = it[1][0] if it[0] == "op" else "dma"
                kk = it[2]
                reads, writes = kk.get("reads", ()), kk.get("writes", ())
                ready = t0
                if acck in writes:
                    ready = max(ready, barrier)
                for key in list(reads) + list(writes):
                    w = lw.get(key)
                    if w is not None:
                        ready = max(ready, w[1] + (lat_from(w[0]) if w[0] != eng else 0.0))
                for key in writes:
                    for (re_, rt_) in lr.get(key, ()):
                        ready = max(ready, rt_ + (lat_from(re_) if re_ != eng else 0.0))
                if eng == "dve":
                    while gcnt.get(int(ready), 0) >= BUDGET:
                        ready = float(int(ready) + 1)
                    gcnt[int(ready)] = gcnt.get(int(ready), 0) + 1
                times.append(ready)
                for key in reads:
                    lr.setdefault(key, []).append((eng, ready))
                for key in writes:
                    lw[key] = (eng, ready)
                    lr[key] = []
            order = sorted(range(i_tail), key=lambda i: (times[i], i))
            for i in order:
                pending.append((times[i], t, items[i]))
            order = sorted(range(i_tail, len(items)), key=lambda i: (times[i], i))
            for i in order:
                tails.append((times[i] + TAIL_DELAY, t, items[i]))
            floor_t[0] = max(times[:i_tail]) if i_tail else t0

        def emit_item(it):
            getattr(S, it[0])(*it[1], **it[2])

        def pop_ready(limit_tile=None):
            while True:
                progressed = False
                if pending and (pending[0][0] <= G[0] if limit_tile is None else pending[0][1] <= limit_tile):
                    it = pending[0][2]
                    if any(k_ in TOPK_KEYS for k_ in it[2].get("writes", ())):
                        while tails and tails[0][1] < pending[0][1]:
                            emit_item(tails.popleft()[2])
                    emit_item(pending.popleft()[2])
                    progressed = True
                if tails and (tails[0][0] <= G[0] if limit_tile is None else tails[0][1] <= limit_tile):
                    if not (pending and pending[0][1] <= tails[0][1]):
                        emit_item(tails.popleft()[2])
                        progressed = True
                if not progressed:
                    break

        def emit_CD(tt):
            p = tt % 2
            ACC, ACCk = acc[p], f"acc{p}"
            N2, N2k = n2[p], f"n2_{p}"
            IDX, IDXk = idx[p], f"idx{p}"
            GT, GTk = gates[p], f"gates{p}"
            r0 = tt * 128
            G[0] = tt * 128

            def advance():
                pop_ready()
                G[0] += 1

            for gi in range(128 // GRP):
                slots = []
                for jj in range(GRP):
                    j = gi * GRP + jj
                    b = rctr[0] % NR
                    rctr[0] += 1
                    slots.append((j, b))
                    rs = ring[:, b * 2048:(b + 1) * 2048]
                    S.dma("pool", lambda h, rs=rs, j=j: h.indirect_dma_start(
                        out=rs, out_offset=None, in_=tbl_d,
                        in_offset=bass.IndirectOffsetOnAxis(ap=IDX[:, j:j + 1], axis=0)),
                        f"g{b}", reads=([IDXk] + TBL_KEYS) if tt == 0 and j == 0 else [IDXk],
                        writes=[f"ring{b}", "stg0", "stg1"] if tt == 0 and j < NR else [f"ring{b}"])
                    S.op("dve", lambda h, rs=rs, j=j: h.scalar_tensor_tensor(
                        out=junk_d, in0=rs[:, 0:1024], scalar=1.0, in1=N2[:], op0=ALU.mult, op1=ALU.mult,
                        accum_out=hh[:, j:j + 1]),
                        reads=[f"ring{b}", N2k], writes=["jd", f"hh{gi}"])
                    advance()
                j0 = gi * GRP
                S.op("act", lambda h, j0=j0: h.activation(out=hh[:, j0:j0 + GRP], in_=hh[:, j0:j0 + GRP],
                                                          func=AF.Gelu_apprx_tanh), reads=[f"hh{gi}"], writes=[f"hh{gi}"])
                for (j, b) in slots:
                    di = dctr[0] % NDG
                    dctr[0] += 1
                    S.op("act", lambda h, j=j: h.activation(out=aw[:, j:j + 1], in_=hh[:, j:j + 1], func=AF.Copy,
                                                            scale=GT[:, j:j + 1]),
                         reads=[f"hh{gi}", GTk], writes=["aw"])
                    S.op("act", lambda h, j=j, di=di: h.activation(out=dg[:, di, :], in_=ident[:], func=AF.Copy,
                                                                   scale=aw[:, j:j + 1]),
                         reads=["ident", "aw"], writes=[f"dg{di}"])
                    for half in range(2):
                        S.op("pe", lambda h, j=j, b=b, di=di, half=half: h.matmul(
                            out=pacc[half][:], lhsT=dg[:, di, :],
                            rhs=ring[:, b * 2048 + 1024 + half * 512:b * 2048 + 1024 + (half + 1) * 512],
                            start=(j == 0), stop=(j == 127)),
                            reads=[f"dg{di}", f"ring{b}"], writes=[f"pacc{half}"])
            pop_ready(limit_tile=tt + 1)
            for half in range(2):
                S.op("dve", lambda h, half=half: h.tensor_tensor(
                    out=ACC[:, half * 512:(half + 1) * 512], in0=pacc[half][:], in1=ACC[:, half * 512:(half + 1) * 512],
                    op=ALU.add), reads=[f"pacc{half}", ACCk], writes=[ACCk])
            S.op("act", lambda h: h.activation(out=junk_a[:], in_=ACC[:], func=AF.Square, accum_out=sm3[:, 0:1]),
                 reads=[ACCk], writes=["junk_a", "sm3"])
            S.op("act", lambda h: h.activation(out=sm3[:, 1:2], in_=sm3[:, 0:1], func=AF.Ln, scale=1.0 / D, bias=EPS),
                 reads=["sm3"], writes=["sm3"])
            S.op("act", lambda h: h.activation(out=sm3[:, 1:2], in_=sm3[:, 1:2], func=AF.Exp, scale=-0.5),
                 reads=["sm3"], writes=["sm3"])
            S.op("dve", lambda h: h.scalar_tensor_tensor(out=N2[:], in0=ACC[:], scalar=sm3[:, 1:2], in1=cap("gFbc"),
                                                         op0=ALU.mult, op1=ALU.mult),
                 reads=[ACCk, "sm3", "cA"], writes=[N2k])
            S.dma("sp", lambda h: h.dma_start(out=y_d[r0:r0 + 128, :], in_=N2[:]), "sty", reads=[N2k],
                  writes=["ydram"])

        items = build_AB(0)
        run_items(items, 0, len(items))
        if NT > 1:
            enqueue_AB(1, 0)
        for tt in range(NT):
            if tt + 2 < NT:
                enqueue_AB(tt + 2, (tt + 1) * 128 - EARLY)
            emit_CD(tt)

        sp = S.E["sp"]
        for key, d in S.dsem.items():
            if key == "sty" or key.startswith("tap_"):
                sp.prog.append(lambda h, d=d: h.wait_ge(d[0], d[1]))
        S.emit()
    return nc, tap_d


def _host_layout(inp):
    f = np.float32
    w_in = np.asarray(inp["w_in"], f)[0]
    wq = np.asarray(inp["peer_wq"], f)[0]
    wall = np.empty((NWALL, 128, 4096), f)

    def blockify(w):
        kc = w.shape[0] // 128
        return w.reshape(kc, 128, w.shape[1]).transpose(1, 0, 2).reshape(128, kc * w.shape[1])

    for b, c0 in enumerate(WIN_COL0):
        wall[b] = blockify(w_in[:, c0:c0 + 512])
    for b in range(4):
        wall[10 + b] = blockify(wq[:, b * 512:(b + 1) * 512])
    wall[14] = blockify(np.asarray(inp["w_branch_a"], f)[0])
    wall[15] = blockify(np.asarray(inp["w_branch_b"], f)[0])
    wo = np.asarray(inp["w_out"], f)[0]
    wall[16] = blockify(wo[0:512])
    wall[17] = blockify(wo[512:1024])

    cA = np.zeros((128, NCA), f)

    def put(name, arr):
        a, b = CA[name]
        cA[:arr.shape[0], a:b] = arr

    tri = np.zeros((128, 128), f)
    ci = np.zeros((128, 2), f)
    for cp in range(128):
        ci[cp, cp // 64] = -1.0 / 16.0
        for c in range(128):
            if cp // 64 == c // 64 and cp > c:
                tri[cp, c] = -1.0 / 16.0
    put("trirev", tri)
    put("chunkind", ci)
    put("iota16", np.broadcast_to(np.arange(16, dtype=f)[None], (128, 16)))
    put("g1T", np.asarray(inp["norm1_g"], f)[0].reshape(8, 128).T)
    put("g2T", np.asarray(inp["norm2_g"], f)[0].reshape(8, 128).T)
    put("goutT", np.asarray(inp["gla_norm_g"], f)[0].reshape(4, 128).T)
    put("sbT", np.asarray(inp["sgu_b"], f)[0].T)
    put("lng", np.broadcast_to(np.asarray(inp["sgu_ln_g"], f)[0][None], (128, 512)))
    put("lnb", np.broadcast_to(np.asarray(inp["sgu_ln_b"], f)[0][None], (128, 512)))
    put("g2bc", np.broadcast_to(np.asarray(inp["norm2_g"], f)[0][None], (128, 1024)))
    put("gFbc", np.broadcast_to(np.asarray(inp["final_g"], f)[None], (128, 1024)))
    put("wga", np.concatenate([np.asarray(inp["w_gate_up"], f)[0], np.asarray(inp["b_gate"], f)], axis=0))

    cB = np.zeros((128, NCB), f)

    def putb(name, arr):
        a, b = CB[name]
        cB[:, a:b] = arr

    putb("ident", np.eye(128, dtype=f))
    putb("k1T", np.asarray(inp["peer_k1"], f)[0].T)
    putb("k2T", np.asarray(inp["peer_k2"], f)[0].T)
    putb("swT", np.asarray(inp["sgu_w"], f)[0].transpose(2, 0, 1).reshape(128, 512))
    pos = np.arange(128) // 64
    putb("mask", (pos[None, :] >= pos[:, None]).astype(f))
    putb("walr", blockify(w_in[:, 2048:2064]))
    return wall, cA, cB


_NC_CACHE = {}


def kernel(**inputs):
    x = np.ascontiguousarray(np.asarray(inputs["x"], np.float32))
    wall, cA, cB = _host_layout(inputs)
    puv = np.concatenate([np.asarray(inputs["peer_u"], np.float32)[0],
                          np.asarray(inputs["peer_v"], np.float32)[0]], axis=1)
    if "nc" not in _NC_CACHE:
        _NC_CACHE["nc"] = build_nc(SEQ // 128)[0]
    nc = _NC_CACHE["nc"]
    in_maps = [{"x": x[b], "wall": wall, "cA": cA, "cB": cB, "peer_uv": puv} for b in range(NCORES)]
    res = run_bass_kernel_spmd(nc, in_maps, core_ids=list(range(NCORES)))
    return np.stack([np.asarray(r["y"], np.float32) for r in res.results], axis=0)
```

```python
import numpy as np
from contextlib import ExitStack
from collections import deque
import concourse.bass as bass
import concourse.mybir as mybir
from concourse.bass_utils import run_bass_kernel_spmd

F32 = mybir.dt.float32
BF16 = mybir.dt.bfloat16
I32 = mybir.dt.int32
U32 = mybir.dt.uint32
AF = mybir.ActivationFunctionType
ALU = mybir.AluOpType
AX = mybir.AxisListType

D = 1024
SEQ = 4096
NCORES = 8
EPS = 1e-6
NEXP = 16384
NBLK = 14
NWALL = 18
WIN_COL0 = [0, 512, 1024, 1536, 2064, 2576, 3088, 3600, 4112, 4624]

CA = {}
_o = 0
for _n, _w in [("trirev", 128), ("chunkind", 2), ("iota16", 16), ("g1T", 8), ("g2T", 8), ("goutT", 4),
               ("sbT", 4), ("lng", 512), ("lnb", 512), ("g2bc", 1024), ("gFbc", 1024), ("wga", 512)]:
    CA[_n] = (_o, _o + _w)
    _o += _w
NCA = _o
CB = {}
_o = 0
for _n, _w in [("ident", 128), ("k1T", 128), ("k2T", 128), ("swT", 512), ("mask", 128), ("walr", 128)]:
    CB[_n] = (_o, _o + _w)
    _o += _w
NCB = _o


class Tok:
    __slots__ = ("sem", "key", "val", "eng")

    def __init__(self, sem, key, val, eng):
        self.sem, self.key, self.val, self.eng = sem, key, val, eng


class Eng:
    def __init__(self, name, sem):
        self.name, self.sem = name, sem
        self.n = 0
        self.seen = {}
        self.prog = []


class Sched:
    ENGS = ("pe", "act", "dve", "pool", "sp")

    def __init__(self, nc, stack):
        self.nc = nc
        self.stack = stack
        self.E = {}
        for n in self.ENGS:
            sem = stack.enter_context(nc.semaphore("e_" + n))
            self.E[n] = Eng(n, sem)
        self.dsem = {}
        self.lastw = {}
        self.readers = {}

    def _dsem(self, key):
        if key not in self.dsem:
            sem = self.stack.enter_context(self.nc.semaphore("d_" + key))
            self.dsem[key] = [sem, 0]
        return self.dsem[key]

    def _wait(self, eng, tok):
        if eng.seen.get(tok.key, 0) >= tok.val:
            return
        eng.seen[tok.key] = tok.val
        eng.prog.append(lambda h, s=tok.sem, v=tok.val: h.wait_ge(s, v))

    def op(self, engname, fn, reads=(), writes=()):
        eng = self.E[engname]
        for k in reads:
            w = self.lastw.get(k)
            if w is not None and not (w.eng is eng and engname == "pe"):
                self._wait(eng, w)
        for k in writes:
            w = self.lastw.get(k)
            if w is not None and not (w.eng is eng and engname == "pe"):
                self._wait(eng, w)
            for r in self.readers.get(k, {}).values():
                if not (r.eng is eng and engname == "pe"):
                    self._wait(eng, r)
        eng.n += 1
        tok = Tok(eng.sem, "e_" + engname, eng.n, eng)
        eng.prog.append(lambda h, f=fn, s=eng.sem: f(h).then_inc(s, 1))
        self._record(tok, reads, writes)
        return tok

    def dma(self, engname, fn, semkey, reads=(), writes=()):
        eng = self.E[engname]
        for k in reads:
            w = self.lastw.get(k)
            if w is not None:
                self._wait(eng, w)
        for k in writes:
            w = self.lastw.get(k)
            if w is not None:
                self._wait(eng, w)
            for r in self.readers.get(k, {}).values():
                self._wait(eng, r)
        d = self._dsem(semkey)
        d[1] += 16
        tok = Tok(d[0], "d_" + semkey, d[1], None)
        eng.prog.append(lambda h, f=fn, s=d[0]: f(h).then_inc(s, 16))
        self._record(tok, reads, writes)
        return tok

    def _record(self, tok, reads, writes):
        for k in reads:
            self.readers.setdefault(k, {})[tok.key] = tok
        for k in writes:
            self.lastw[k] = tok
            self.readers[k] = {}

    def emit(self):
        hmap = {"pe": "tensor", "act": "scalar", "dve": "vector", "pool": "gpsimd", "sp": "sync"}
        with self.nc.Block() as block:
            for n in self.ENGS:
                prog = self.E[n].prog

                def body(h, prog=prog):
                    for t in prog:
                        t(h)

                getattr(block, hmap[n])(body)


class Defer:
    def __init__(self):
        self.items = []

    def op(self, *a, **k):
        self.items.append(("op", a, k))

    def dma(self, *a, **k):
        self.items.append(("dma", a, k))


def build_nc(NT=32, taps=None):
    taps = taps or []
    nc = bass.Bass("TRN2", target_bir_lowering=False)
    x_d = nc.dram_tensor("x", [SEQ, D], F32, kind="ExternalInput").ap()
    wall_d = nc.dram_tensor("wall", [NWALL, 128, 4096], F32, kind="ExternalInput").ap()
    cA_d = nc.dram_tensor("cA", [128, NCA], F32, kind="ExternalInput").ap()
    cB_d = nc.dram_tensor("cB", [128, NCB], F32, kind="ExternalInput").ap()
    puv_d = nc.dram_tensor("peer_uv", [NEXP, 2 * D], F32, kind="ExternalInput").ap()
    y_d = nc.dram_tensor("y", [SEQ, D], F32, kind="ExternalOutput").ap()
    wsc_d = nc.dram_tensor("wsc", [NBLK, 128, 4096], BF16, kind="Internal").ap()
    tbl_d = nc.dram_tensor("tbl", [NEXP, 2 * D], BF16, kind="Internal").ap()
    tap_d = {}

    with ExitStack() as st:
        st.enter_context(nc.allow_low_precision("bf16 matmul operands, fp32 accumulation"))
        S = Sched(nc, st)

        sb_total = [0]

        def sb(name, shape, dt):
            n = 1
            for d_ in shape[1:]:
                n *= d_
            sb_total[0] += n * (2 if dt == BF16 else 4)
            return st.enter_context(nc.sbuf_tensor(name, shape, dt))

        def ps(name, shape, dt):
            return st.enter_context(nc.psum_tensor(name, shape, dt))

        cA = sb("cA_sb", [128, NCA], F32)
        ident = sb("ident", [128, 128], BF16)
        k1T = sb("k1T", [128, 128], BF16)
        k2T = sb("k2T", [128, 128], BF16)
        swT = sb("swT", [128, 4, 128], BF16)
        walr = sb("walr", [128, 8, 16], BF16)
        wa = sb("wa", [128, 4, 1024], BF16)
        wb = sb("wb", [128, 4, 1024], BF16)
        wo = sb("wo", [128, 8, 1024], BF16)
        NWB = 2
        wbuf = [sb(f"wbuf{i}", [128, 8, 512], BF16) for i in range(NWB)]
        NR = 15
        ring = sb("ring", [128, NR * 2048], BF16)
        NDG = 8
        dg = sb("dg", [128, NDG, 128], BF16)
        acc = [sb(f"acc{i}", [128, 1024], F32) for i in range(2)]
        state = sb("state", [128, 512], F32)
        state_bf = [sb(f"state_bf{i}", [128, 512], BF16) for i in range(2)]
        junk_a = sb("junk_a", [128, 1024], BF16)
        jd_raw = sb("jd_raw", [128, 512], F32)
        junk_d = jd_raw[:].bitcast(BF16)
        s_wk_t = sb("s_wk", [128, 256], F32)
        s_wk = s_wk_t[:]
        xn = sb("xn", [128, 1024], BF16)
        nT = sb("nT", [128, 8, 128], BF16)
        qT = sb("qT", [128, 512], BF16)
        k_sb = sb("k_sb", [128, 512], F32)
        v_bf = sb("v_bf", [128, 512], BF16)
        silu_r = sb("silu_r", [128, 512], F32)
        sugv = sb("sugv", [128, 1024], F32)
        sga = sb("sga", [128, 1024], F32)
        sgb = sb("sgb", [128, 1024], F32)
        xs = sga
        xs2 = sugv
        alT = sb("alT", [32, 128], F32)
        Lg = sb("Lg", [128, 512], F32)
        Ek = sb("Ek", [128, 512], F32)
        dec = sb("dec", [128, 8], F32)
        kdec = sb("kdec", [128, 512], BF16)
        ya = Lg
        ya_bf = sb("ya_bf", [128, 512], BF16)
        yaT = sb("yaT", [128, 4, 128], BF16)
        vn = Ek
        vn_bf = sb("vn_bf", [128, 512], BF16)
        yb = sb("yb", [128, 512], BF16)
        ybT = sb("ybT", [128, 4, 128], BF16)
        mgq = sb("mgq", [128, 2048], BF16)
        mg_bf = mgq[:, 0:1024]
        mgT = mgq[:, 1024:2048].rearrange("p (c t) -> p c t", c=8)
        qpT = mgq[:].rearrange("p (g t) -> p g t", g=16)
        n2 = [sb(f"n2_{i}", [128, 1024], F32) for i in range(2)]
        s_sb = sb("s_sb", [128, 16, 128], F32)
        V16 = sb("V16", [128, 16, 16], F32)
        I16 = sb("I16", [128, 16, 16], U32)
        VAL = sb("VAL", [128, 8, 16], F32)
        CI = sb("CI", [128, 8, 16], U32)
        IA = sb("IA", [128, 8, 16], U32)
        IB = sb("IB", [128, 8, 16], U32)
        IAf = sb("IAf", [128, 8, 16], F32)
        IBf = sb("IBf", [128, 8, 16], F32)
        E1 = sb("E1", [128, 8, 16], F32)
        E2 = sb("E2", [128, 8, 16], F32)
        idxf = sb("idxf", [128, 128], F32)
        idx = [sb(f"idx{i}", [128, 128], I32) for i in range(2)]
        ge = sb("ge", [128, 8, 16], F32)
        gs = sb("gs", [128, 8], F32)
        gates = [sb(f"gates{i}", [128, 128], F32) for i in range(2)]
        hh = sb("hh", [128, 128], F32)
        gl = sb("gl", [128, 128], F32)
        aw = sb("aw", [128, 128], F32)
        sm1 = sb("sm1", [128, 2], F32)
        smo = sb("smo", [128, 8], F32)
        smln = sb("smln", [128, 1], F32)
        sm2 = sb("sm2", [128, 2], F32)
        sm3 = sb("sm3", [128, 2], F32)
        ghalf = sb("ghalf", [128, 4], F32)
        bnst = sb("bnst", [128, 6], F32)
        bnag = sb("bnag", [128, 2], F32)

        tp = ps("tp", [128, 1024], BF16)
        pacc = [ps(f"pacc{i}", [128, 512], F32) for i in range(2)]
        NPB = 5
        pbank = [ps(f"pb{i}", [128, 512], F32) for i in range(NPB)]
        pctr = [0]

        def bank():
            i = pctr[0] % NPB
            pctr[0] += 1
            return pbank[i], f"pb{i}"

        def cap(name):
            a, b = CA[name]
            return cA[:, a:b]

        S.dma("sp", lambda h: h.dma_start(out=cA[:], in_=cA_d), "ldc", writes=["cA"])
        stg = ring[:].bitcast(F32)
        S.dma("sp", lambda h: h.dma_start(out=stg[:, 0:NCB], in_=cB_d), "ldc2", writes=["stg0"])

        def cbp(name):
            a, b = CB[name]
            return stg[:, a:b]

        S.op("dve", lambda h: h.memset(alT[:], 1.0), writes=["alT"])
        S.op("dve", lambda h: h.tensor_scalar(out=ghalf[:], in0=cap("goutT"), scalar1=0.5, scalar2=None, op0=ALU.mult),
             reads=["cA"], writes=["ghalf"])
        S.op("dve", lambda h: h.memset(state[:], 0.0), writes=["state"])
        S.op("dve", lambda h: h.tensor_copy(out=ident[:], in_=cbp("ident")), reads=["stg0"], writes=["ident"])
        S.op("dve", lambda h: h.tensor_copy(out=k1T[:], in_=cbp("k1T")), reads=["stg0"], writes=["k1T"])
        S.op("dve", lambda h: h.tensor_copy(out=k2T[:], in_=cbp("k2T")), reads=["stg0"], writes=["k2T"])
        S.op("dve", lambda h: h.tensor_copy(out=walr[:].rearrange("p c n -> p (c n)"), in_=cbp("walr")),
             reads=["stg0"], writes=["walr"])
        msk = cbp("mask")
        S.op("dve", lambda h: h.tensor_tensor(
            out=swT[:], in0=cbp("swT").rearrange("p (g i) -> p g i", g=4),
            in1=msk.unsqueeze(1).to_broadcast([128, 4, 128]), op=ALU.mult),
            reads=["stg0"], writes=["swT"])
        for b in range(NBLK):
            S.dma("pool", lambda h, b=b: h.dma_start(out=wsc_d[b], in_=wall_d[b]), f"wcv{b}", writes=[f"wsc{b}"])
        S.dma("pool", lambda h: h.dma_start(out=wa[:].rearrange("p c n -> p (c n)"), in_=wall_d[14]), "wcva", writes=["wa"])
        S.dma("pool", lambda h: h.dma_start(out=wb[:].rearrange("p c n -> p (c n)"), in_=wall_d[15]), "wcvb", writes=["wb"])
        for hb in range(2):
            S.dma("pool", lambda h, hb=hb: h.dma_start(
                out=wo[:, hb * 4:hb * 4 + 4, :].rearrange("p c n -> p (c n)"), in_=wall_d[16 + hb]),
                f"wcvo{hb}", writes=["wo"])
        NCH = 32
        RCH = NEXP // NCH
        TBL_KEYS = [f"tbl{i}" for i in range(NCH)]
        for i in range(NCH):
            S.dma("pool", lambda h, i=i: h.dma_start(out=tbl_d[i * RCH:(i + 1) * RCH, :],
                                                     in_=puv_d[i * RCH:(i + 1) * RCH, :]),
                  "tcv", writes=[TBL_KEYS[i]])

        wctr = [0]
        rctr = [0]
        dctr = [0]
        ring_alias = {b: [f"stg{min(b * 2048 * 2 // (4096 * 4), 1)}"] for b in range(NR)}

        def build_AB(tt):
            Q = Defer()
            p = tt % 2
            ACC, ACCk = acc[p], f"acc{p}"
            N2, N2k = n2[p], f"n2_{p}"
            IDX, IDXk = idx[p], f"idx{p}"
            GT, GTk = gates[p], f"gates{p}"
            r0 = tt * 128
            T0 = (tt == 0)

            def tap(name, ap, key, shape):
                if name not in taps or not T0:
                    return
                t = nc.dram_tensor("tap_" + name, list(shape), ap.dtype, kind="ExternalOutput").ap()
                tap_d[name] = t
                Q.dma("sp", lambda h: h.dma_start(out=t, in_=ap), "tap_" + name, reads=[key])

            def rstd_from_ss(ss_ap, out_ap, n, key):
                Q.op("act", lambda h: h.activation(out=out_ap, in_=ss_ap, func=AF.Ln, scale=1.0 / n, bias=EPS),
                     reads=[key], writes=[key])
                Q.op("act", lambda h: h.activation(out=out_ap, in_=out_ap, func=AF.Exp, scale=-0.5),
                     reads=[key], writes=[key])

            def transposes(src_tile, nchunks, srckey):
                for c in range(nchunks):
                    Q.op("pe", lambda h, c=c: h.transpose(out=tp[:, c * 128:(c + 1) * 128],
                                                         in_=src_tile[:, c * 128:(c + 1) * 128], identity=ident[:]),
                         reads=[srckey, "ident"], writes=["tp"])

            def wload(blk):
                i = wctr[0] % NWB
                wctr[0] += 1
                Q.dma("sp", lambda h, i=i, blk=blk: h.dma_start(out=wbuf[i][:].rearrange("p c n -> p (c n)"),
                                                                in_=wsc_d[blk]),
                      f"wl{i}", reads=[f"wsc{blk}"], writes=[f"wbuf{i}"])
                return wbuf[i], f"wbuf{i}"

            if tt == 0:
                Q.dma("sp", lambda h: h.dma_start(out=xs[:], in_=x_d[0:128, :]), "ldx", writes=["sga"])
            Q.op("act", lambda h: h.activation(out=junk_a[:], in_=xs[:], func=AF.Square, accum_out=sm1[:, 0:1]),
                 reads=["sga"], writes=["junk_a", "sm1"])
            rstd_from_ss(sm1[:, 0:1], sm1[:, 1:2], D, "sm1")
            Q.op("act", lambda h: h.activation(out=xn[:], in_=xs[:], func=AF.Copy, scale=sm1[:, 1:2]),
                 reads=["sga", "sm1"], writes=["xn"])
            transposes(xn, 8, "xn")
            Q.op("dve", lambda h: h.tensor_tensor(out=nT[:], in0=tp[:].rearrange("p (c t) -> p c t", c=8),
                                                  in1=cap("g1T").unsqueeze(2).to_broadcast([128, 8, 128]),
                                                  op=ALU.mult),
                 reads=["tp", "cA"], writes=["nT"])

            w, wk = wload(0)
            pq, pqk = bank()
            for hd in range(4):
                for c in range(8):
                    Q.op("pe", lambda h, hd=hd, c=c, w=w, pq=pq: h.matmul(
                        out=pq[:, hd * 128:(hd + 1) * 128], lhsT=w[:, c, hd * 128:(hd + 1) * 128],
                        rhs=nT[:, c, :], start=(c == 0), stop=(c == 7)),
                        reads=[wk, "nT"], writes=[pqk])
            Q.op("act", lambda h, pq=pq: h.activation(out=qT[:], in_=pq[:], func=AF.Copy, scale=128.0 ** -0.5),
                 reads=[pqk], writes=["qT"])

            def proj_block(blk):
                w, wk = wload(blk)
                pb, pbk = bank()
                for c in range(8):
                    Q.op("pe", lambda h, c=c, w=w, pb=pb: h.matmul(out=pb[:], lhsT=nT[:, c, :], rhs=w[:, c, :],
                                                                  start=(c == 0), stop=(c == 7)),
                         reads=[wk, "nT"], writes=[pbk])
                return pb, pbk

            pa, pak = bank()
            for c in range(8):
                Q.op("pe", lambda h, c=c, pa=pa: h.matmul(out=pa[0:16, 0:128], lhsT=walr[:, c, :], rhs=nT[:, c, :],
                                                         start=(c == 0), stop=(c == 7)),
                     reads=["walr", "nT"], writes=[pak])
            Q.op("act", lambda h, pa=pa: h.activation(out=alT[0:16, :], in_=pa[0:16, 0:128], func=AF.Copy),
                 reads=[pak], writes=["alT"])
            pz, pzk = bank()
            Q.op("pe", lambda h, pz=pz: h.matmul(out=pz[:], lhsT=alT[0:17, :], rhs=cap("wga")[0:17, :],
                                                 start=True, stop=True),
                 reads=["alT", "cA"], writes=[pzk])
            Q.op("act", lambda h, pz=pz: h.activation(out=Lg[:], in_=pz[:], func=AF.Exp, scale=-1.0),
                 reads=[pzk], writes=["Lg"])
            Q.op("act", lambda h: h.activation(out=Lg[:], in_=Lg[:], func=AF.Ln, bias=1.0, scale=1.0),
                 reads=["Lg"], writes=["Lg"])
            pb, pbk = proj_block(1)
            Q.op("act", lambda h, pb=pb: h.activation(out=k_sb[:], in_=pb[:], func=AF.Copy), reads=[pbk], writes=["k_sb"])
            pb, pbk = proj_block(2)
            Q.op("act", lambda h, pb=pb: h.activation(out=v_bf[:], in_=pb[:], func=AF.Copy), reads=[pbk], writes=["v_bf"])
            prv, prvk = bank()
            Q.op("pe", lambda h, prv=prv: h.matmul(out=prv[:], lhsT=cap("trirev"), rhs=Lg[:], start=True, stop=True),
                 reads=["cA", "Lg"], writes=[prvk])
            ptt, pttk = bank()
            for hd in range(4):
                Q.op("pe", lambda h, hd=hd, ptt=ptt: h.matmul(out=ptt[:, hd * 2:hd * 2 + 2],
                                                              lhsT=Lg[:, hd * 128:(hd + 1) * 128],
                                                              rhs=cap("chunkind"), start=True, stop=True),
                     reads=["cA", "Lg"], writes=[pttk])
            Q.op("act", lambda h, prv=prv: h.activation(out=Ek[:], in_=prv[:], func=AF.Exp), reads=[prvk], writes=["Ek"])
            Q.op("act", lambda h, ptt=ptt: h.activation(out=dec[:], in_=ptt[:, 0:8], func=AF.Exp), reads=[pttk], writes=["dec"])
            Q.op("dve", lambda h: h.tensor_tensor(out=kdec[:], in0=k_sb[:], in1=Ek[:], op=ALU.mult),
                 reads=["k_sb", "Ek"], writes=["kdec"])
            pb, pbk = proj_block(3)
            Q.op("act", lambda h, pb=pb: h.activation(out=silu_r[:], in_=pb[:], func=AF.Tanh, scale=0.5), reads=[pbk], writes=["silu_r"])
            Q.op("dve", lambda h, pb=pb: h.scalar_tensor_tensor(out=silu_r[:], in0=silu_r[:], scalar=1.0, in1=pb[:],
                                                                op0=ALU.add, op1=ALU.mult),
                 reads=[pbk, "silu_r"], writes=["silu_r"])
            for i, blk in enumerate((4, 5)):
                pb, pbk = proj_block(blk)
                Q.op("act", lambda h, pb=pb, i=i: h.activation(out=sugv[:, i * 512:(i + 1) * 512], in_=pb[:],
                                                               func=AF.Gelu_apprx_tanh),
                     reads=[pbk], writes=["sugv"])
            for i, blk in enumerate((6, 7)):
                pb, pbk = proj_block(blk)
                Q.op("act", lambda h, pb=pb, i=i: h.activation(out=sga[:, i * 512:(i + 1) * 512], in_=pb[:], func=AF.Tanh, scale=0.5),
                     reads=[pbk], writes=["sga"])
            for i, blk in enumerate((8, 9)):
                pb, pbk = proj_block(blk)
                Q.op("act", lambda h, pb=pb, i=i: h.activation(out=sgb[:, i * 512:(i + 1) * 512], in_=pb[:], func=AF.Tanh, scale=0.5),
                     reads=[pbk], writes=["sgb"])

            po, pok = bank()
            for j in range(2):
                pkv, pkvk = bank()
                for hd in range(4):
                    Q.op("pe", lambda h, hd=hd, j=j, pkv=pkv: h.matmul(
                        out=pkv[:, hd * 128:(hd + 1) * 128],
                        lhsT=kdec[64 * j:64 * j + 64, hd * 128:(hd + 1) * 128],
                        rhs=v_bf[64 * j:64 * j + 64, hd * 128:(hd + 1) * 128], start=True, stop=True),
                        reads=["kdec", "v_bf"], writes=[pkvk])
                for hd in range(4):
                    Q.op("dve", lambda h, hd=hd, j=j, pkv=pkv: h.scalar_tensor_tensor(
                        out=state[:, hd * 128:(hd + 1) * 128], in0=state[:, hd * 128:(hd + 1) * 128],
                        scalar=dec[:, hd * 2 + j:hd * 2 + j + 1], in1=pkv[:, hd * 128:(hd + 1) * 128],
                        op0=ALU.mult, op1=ALU.add),
                        reads=[f"state{hd}", "dec", pkvk], writes=[f"state{hd}"])
                Q.op("act", lambda h, j=j: h.activation(out=state_bf[j][:], in_=state[:], func=AF.Copy),
                     reads=[f"state{hd}" for hd in range(4)], writes=[f"state_bf{j}"])
                for hd in range(4):
                    Q.op("pe", lambda h, hd=hd, j=j, po=po: h.matmul(
                        out=po[64 * j:64 * j + 64, hd * 128:(hd + 1) * 128],
                        lhsT=qT[:, hd * 128 + 64 * j:hd * 128 + 64 * j + 64],
                        rhs=state_bf[j][:, hd * 128:(hd + 1) * 128], start=True, stop=True),
                        reads=["qT", f"state_bf{j}"], writes=[pok])
            for hd in range(4):
                Q.op("act", lambda h, hd=hd, po=po: h.activation(out=junk_a[:, hd * 128:(hd + 1) * 128],
                                                                 in_=po[:, hd * 128:(hd + 1) * 128], func=AF.Square,
                                                                 accum_out=smo[:, hd:hd + 1]),
                     reads=[pok], writes=["junk_a", "smo"])
            rstd_from_ss(smo[:, 0:4], smo[:, 4:8], 128, "smo")
            Q.op("dve", lambda h, po=po: h.tensor_tensor(out=ya[:].rearrange("p (g v) -> p g v", g=4),
                                                         in0=po[:].rearrange("p (g v) -> p g v", g=4),
                                                         in1=smo[:, 4:8].unsqueeze(2).to_broadcast([128, 4, 128]),
                                                         op=ALU.mult),
                 reads=[pok, "smo"], writes=["Lg"])
            Q.op("dve", lambda h: h.tensor_tensor(out=ya_bf[:], in0=ya[:], in1=silu_r[:], op=ALU.mult),
                 reads=["Lg", "silu_r"], writes=["ya_bf"])
            transposes(ya_bf, 4, "ya_bf")
            Q.op("dve", lambda h: h.tensor_tensor(out=yaT[:], in0=tp[:, 0:512].rearrange("p (c t) -> p c t", c=4),
                                                  in1=ghalf[:].unsqueeze(2).to_broadcast([128, 4, 128]),
                                                  op=ALU.mult),
                 reads=["tp", "ghalf"], writes=["yaT"])

            svg = sugv[:, 512:1024]
            Q.op("dve", lambda h: h.bn_stats(out=bnst[:], in_=svg), reads=["sugv"], writes=["bnst"])
            Q.op("dve", lambda h: h.bn_aggr(out=bnag[:], in_=bnst[:]), reads=["bnst"], writes=["bnag"])
            Q.op("act", lambda h: h.activation(out=smln[:], in_=bnag[:, 1:2], func=AF.Ln, scale=1.0, bias=EPS),
                 reads=["bnag"], writes=["smln"])
            Q.op("act", lambda h: h.activation(out=smln[:], in_=smln[:], func=AF.Exp, scale=-0.5),
                 reads=["smln"], writes=["smln"])
            Q.op("dve", lambda h: h.tensor_scalar(out=vn[:], in0=svg, scalar1=bnag[:, 0:1], scalar2=smln[:, 0:1],
                                                  op0=ALU.subtract, op1=ALU.mult),
                 reads=["sugv", "bnag", "smln"], writes=["Ek"])
            Q.op("dve", lambda h: h.tensor_tensor(out=vn[:], in0=vn[:], in1=cap("lng"), op=ALU.mult),
                 reads=["Ek", "cA"], writes=["Ek"])
            Q.op("dve", lambda h: h.tensor_tensor(out=vn_bf[:], in0=vn[:], in1=cap("lnb"), op=ALU.add),
                 reads=["Ek", "cA"], writes=["vn_bf"])
            pm, pmk = bank()
            for g in range(4):
                Q.op("pe", lambda h, g=g, pm=pm: h.matmul(out=pm[:, g * 128:(g + 1) * 128], lhsT=swT[:, g, :],
                                                         rhs=vn_bf[:, g * 128:(g + 1) * 128], start=True, stop=True),
                     reads=["swT", "vn_bf"], writes=[pmk])
            for g in range(4):
                Q.op("dve", lambda h, g=g, pm=pm: h.scalar_tensor_tensor(
                    out=yb[:, g * 128:(g + 1) * 128], in0=pm[:, g * 128:(g + 1) * 128],
                    scalar=cap("sbT")[:, g:g + 1], in1=sugv[:, g * 128:(g + 1) * 128], op0=ALU.add, op1=ALU.mult),
                    reads=[pmk, "cA", "sugv"], writes=["yb"])
            transposes(yb, 4, "yb")
            Q.dma("sp", lambda h: h.dma_start(out=xs2[:], in_=x_d[r0:r0 + 128, :]), "ldx2", writes=["sugv"])
            Q.op("act", lambda h: h.activation(out=ybT[:].rearrange("p c t -> p (c t)"), in_=tp[:, 0:512], func=AF.Copy),
                 reads=["tp"], writes=["ybT"])

            for half in range(2):
                pA, pAk = bank()
                for c in range(4):
                    Q.op("pe", lambda h, c=c, half=half, pA=pA: h.matmul(
                        out=pA[:], lhsT=yaT[:, c, :], rhs=wa[:, c, half * 512:(half + 1) * 512],
                        start=(c == 0), stop=(c == 3)), reads=["yaT", "wa"], writes=[pAk])
                Q.op("dve", lambda h, half=half, pA=pA: h.scalar_tensor_tensor(
                    out=sga[:, half * 512:(half + 1) * 512], in0=sga[:, half * 512:(half + 1) * 512], scalar=1.0,
                    in1=pA[:], op0=ALU.add, op1=ALU.mult), reads=[pAk, "sga"], writes=["sga"])
                pB, pBk = bank()
                for c in range(4):
                    Q.op("pe", lambda h, c=c, half=half, pB=pB: h.matmul(
                        out=pB[:], lhsT=ybT[:, c, :], rhs=wb[:, c, half * 512:(half + 1) * 512],
                        start=(c == 0), stop=(c == 3)), reads=["ybT", "wb"], writes=[pBk])
                Q.op("dve", lambda h, half=half, pB=pB: h.scalar_tensor_tensor(
                    out=sgb[:, half * 512:(half + 1) * 512], in0=sgb[:, half * 512:(half + 1) * 512], scalar=1.0,
                    in1=pB[:], op0=ALU.add, op1=ALU.mult), reads=[pBk, "sgb"], writes=["sgb"])
            Q.op("dve", lambda h: h.tensor_tensor(out=mg_bf, in0=sga[:], in1=sgb[:], op=ALU.add),
                 reads=["sga", "sgb"], writes=["mgq"])
            transposes(mg_bf, 8, "mgq")
            Q.op("act", lambda h: h.activation(out=mgq[:, 1024:2048], in_=tp[:], func=AF.Copy, scale=0.5),
                 reads=["tp"], writes=["mgq"])
            for half in range(2):
                pD, pDk = bank()
                for c in range(8):
                    Q.op("pe", lambda h, c=c, half=half, pD=pD: h.matmul(
                        out=pD[:], lhsT=mgT[:, c, :], rhs=wo[:, c, half * 512:(half + 1) * 512],
                        start=(c == 0), stop=(c == 7)), reads=["mgq", "wo"], writes=[pDk])
                Q.op("dve", lambda h, half=half, pD=pD: h.tensor_tensor(
                    out=ACC[:, half * 512:(half + 1) * 512], in0=pD[:], in1=xs2[:, half * 512:(half + 1) * 512],
                    op=ALU.add), reads=[pDk, "sugv"], writes=[ACCk])
            tap("h", ACC[:], ACCk, [128, 1024])
            if tt + 1 < NT:
                Q.dma("sp", lambda h: h.dma_start(out=xs[:], in_=x_d[r0 + 128:r0 + 256, :]), "ldx", writes=["sga"])

            Q.op("act", lambda h: h.activation(out=junk_a[:], in_=ACC[:], func=AF.Square, accum_out=sm2[:, 0:1]),
                 reads=[ACCk], writes=["junk_a", "sm2"])
            rstd_from_ss(sm2[:, 0:1], sm2[:, 1:2], D, "sm2")
            Q.op("dve", lambda h: h.scalar_tensor_tensor(out=N2[:], in0=ACC[:], scalar=sm2[:, 1:2], in1=cap("g2bc"),
                                                         op0=ALU.mult, op1=ALU.mult),
                 reads=[ACCk, "sm2", "cA"], writes=[N2k])
            Q.op("act", lambda h: h.activation(out=xn[:], in_=N2[:], func=AF.Copy), reads=[N2k], writes=["xn"])
            transposes(xn, 8, "xn")
            Q.op("act", lambda h: h.activation(out=nT[:].rearrange("p c t -> p (c t)"), in_=tp[:], func=AF.Copy),
                 reads=["tp"], writes=["nT"])

            for blk in range(4):
                w, wk = wload(10 + blk)
                pb, pbk = bank()
                for gg in range(4):
                    for c in range(8):
                        Q.op("pe", lambda h, gg=gg, c=c, w=w, pb=pb: h.matmul(
                            out=pb[:, gg * 128:(gg + 1) * 128], lhsT=w[:, c, gg * 128:(gg + 1) * 128],
                            rhs=nT[:, c, :], start=(c == 0), stop=(c == 7)),
                            reads=[wk, "nT"], writes=[pbk])
                Q.op("act", lambda h, blk=blk, pb=pb: h.activation(
                    out=qpT[:, blk * 4:(blk + 1) * 4, :].rearrange("p g t -> p (g t)"), in_=pb[:], func=AF.Copy),
                    reads=[pbk], writes=["mgq"])
            for blk in range(4):
                pb, pbk = bank()
                for gg in range(4):
                    g = blk * 4 + gg
                    kk, kkk = (k1T, "k1T") if g % 2 == 0 else (k2T, "k2T")
                    Q.op("pe", lambda h, g=g, gg=gg, kk=kk, pb=pb: h.matmul(
                        out=pb[:, gg * 128:(gg + 1) * 128], lhsT=qpT[:, g, :], rhs=kk[:], start=True, stop=True),
                        reads=["mgq", kkk], writes=[pbk])
                Q.op("act", lambda h, blk=blk, pb=pb: h.activation(
                    out=s_sb[:, blk * 4:(blk + 1) * 4, :].rearrange("p g t -> p (g t)"), in_=pb[:], func=AF.Copy),
                    reads=[pbk], writes=["s_sb"])

            for g in range(16):
                sg = s_sb[:, g, :]
                Q.op("dve", lambda h, g=g, sg=sg: h.max(out=V16[:, g, 0:8], in_=sg), reads=["s_sb"], writes=["V16"])
                Q.op("dve", lambda h, g=g, sg=sg: h.max_index(out=I16[:, g, 0:8], in_max=V16[:, g, 0:8], in_values=sg),
                     reads=["s_sb", "V16"], writes=["I16"])
                Q.op("dve", lambda h, g=g, sg=sg: h.match_replace(out=s_wk[:, 0:128], in_to_replace=V16[:, g, 0:8],
                                                                  in_values=sg, imm_value=-1e30),
                     reads=["s_sb", "V16"], writes=["s_wk"])
                Q.op("dve", lambda h, g=g: h.max(out=V16[:, g, 8:16], in_=s_wk[:, 0:128]), reads=["s_wk"], writes=["V16"])
                Q.op("dve", lambda h, g=g: h.max_index(out=I16[:, g, 8:16], in_max=V16[:, g, 8:16],
                                                       in_values=s_wk[:, 0:128]),
                     reads=["s_wk", "V16"], writes=["I16"])
            Q.op("dve", lambda h: h.tensor_copy(out=I16[:].bitcast(F32), in_=I16[:]), reads=["I16"], writes=["I16"])
            V16v = V16[:].rearrange("p (h t) r -> p h t r", t=2)
            I16v = I16[:].bitcast(F32).rearrange("p (h t) r -> p h t r", t=2)
            grid = s_sb[:].rearrange("p g n -> p (g n)").rearrange("p (h a b) -> p h a b", h=8, a=16)
            Q.op("dve", lambda h: h.tensor_tensor(out=grid, in0=V16v[:, :, 0, :].unsqueeze(3).to_broadcast([128, 8, 16, 16]),
                                                  in1=V16v[:, :, 1, :].unsqueeze(2).to_broadcast([128, 8, 16, 16]),
                                                  op=ALU.add),
                 reads=["V16", "s_sb"], writes=["s_sb"])
            candf = s_sb[:].rearrange("p g n -> p (g n)").rearrange("p (h c) -> p h c", h=8)
            for hd in range(8):
                ch = candf[:, hd, :]
                Q.op("dve", lambda h, hd=hd, ch=ch: h.max(out=VAL[:, hd, 0:8], in_=ch), reads=["s_sb"], writes=["VAL"])
                Q.op("dve", lambda h, hd=hd, ch=ch: h.max_index(out=CI[:, hd, 0:8], in_max=VAL[:, hd, 0:8], in_values=ch),
                     reads=["s_sb", "VAL"], writes=["CI"])
                Q.op("dve", lambda h, hd=hd, ch=ch: h.match_replace(out=s_wk, in_to_replace=VAL[:, hd, 0:8],
                                                                    in_values=ch, imm_value=-1e30),
                     reads=["s_sb", "VAL"], writes=["s_wk"])
                Q.op("dve", lambda h, hd=hd: h.max(out=VAL[:, hd, 8:16], in_=s_wk), reads=["s_wk"], writes=["VAL"])
                Q.op("dve", lambda h, hd=hd: h.max_index(out=CI[:, hd, 8:16], in_max=VAL[:, hd, 8:16], in_values=s_wk),
                     reads=["s_wk", "VAL"], writes=["CI"])
            Q.op("dve", lambda h: h.tensor_single_scalar(out=IA[:], in_=CI[:], scalar=4, op=ALU.logical_shift_right),
                 reads=["CI"], writes=["IA"])
            Q.op("dve", lambda h: h.tensor_single_scalar(out=IB[:], in_=CI[:], scalar=15, op=ALU.bitwise_and),
                 reads=["CI"], writes=["IB"])
            Q.op("dve", lambda h: h.tensor_copy(out=IAf[:], in_=IA[:]), reads=["IA"], writes=["IAf"])
            Q.op("dve", lambda h: h.tensor_copy(out=IBf[:], in_=IB[:]), reads=["IB"], writes=["IBf"])
            iob = cap("iota16").unsqueeze(1).unsqueeze(1).to_broadcast([128, 8, 16, 16])
            for (If, Ifk, half, Eo, Ek_) in ((IAf, "IAf", 0, E1, "E1"), (IBf, "IBf", 1, E2, "E2")):
                Q.op("dve", lambda h, If=If: h.tensor_tensor(out=grid, in0=If[:].unsqueeze(3).to_broadcast([128, 8, 16, 16]),
                                                             in1=iob, op=ALU.is_equal),
                     reads=[Ifk, "cA"], writes=["s_sb"])
                Q.op("dve", lambda h, half=half: h.tensor_tensor(
                    out=grid, in0=grid, in1=I16v[:, :, half, :].unsqueeze(2).to_broadcast([128, 8, 16, 16]), op=ALU.mult),
                    reads=["s_sb", "I16"], writes=["s_sb"])
                Q.op("dve", lambda h, Eo=Eo: h.tensor_reduce(out=Eo[:], in_=grid, axis=AX.X, op=ALU.add),
                     reads=["s_sb"], writes=[Ek_])
            Q.op("dve", lambda h: h.scalar_tensor_tensor(out=idxf[:], in0=E1[:].rearrange("p h k -> p (h k)"), scalar=128.0,
                                                         in1=E2[:].rearrange("p h k -> p (h k)"), op0=ALU.mult, op1=ALU.add),
                 reads=["E1", "E2"], writes=["idxf"])
            Q.op("dve", lambda h: h.tensor_copy(out=IDX[:], in_=idxf[:]), reads=["idxf"], writes=[IDXk])
            Q.op("dve", lambda h: h.tensor_tensor(out=ge[:], in0=VAL[:], in1=VAL[:, :, 0:1].to_broadcast([128, 8, 16]),
                                                  op=ALU.subtract), reads=["VAL"], writes=["ge"])
            Q.op("act", lambda h: h.activation(out=ge[:], in_=ge[:], func=AF.Exp), reads=["ge"], writes=["ge"])
            Q.op("dve", lambda h: h.tensor_reduce(out=gs[:], in_=ge[:], axis=AX.X, op=ALU.add), reads=["ge"], writes=["gs"])
            Q.op("dve", lambda h: h.reciprocal(out=gs[:], in_=gs[:]), reads=["gs"], writes=["gs"])
            Q.op("dve", lambda h: h.tensor_tensor(out=GT[:].rearrange("p (h k) -> p h k", h=8), in0=ge[:],
                                                  in1=gs[:].unsqueeze(2).to_broadcast([128, 8, 16]), op=ALU.mult),
                 reads=["ge", "gs"], writes=[GTk])
            tap("idxf", idxf[:], "idxf", [128, 128])
            tap("gates", GT[:], GTk, [128, 128])
            return Q.items

        def run_items(items, lo, hi):
            for kind, a, k in items[lo:hi]:
                getattr(S, kind)(*a, **k)

        GRP = 2
        X_LAT = 1.0
        PE_LAT = 2.0
        DMA_LAT = 5.0
        BUDGET = 3
        EARLY = 100
        TAIL_DELAY = 8.0
        TOPK_KEYS = {"s_sb", "V16", "I16", "s_wk", "I16f", "VAL", "CI", "IA", "IB", "IAf", "IBf", "E1", "E2",
                     "idxf", "ge", "gs"}

        pending = deque()
        G = [0]
        floor_t = [0.0]

        def lat_from(eng):
            return {"dma": DMA_LAT, "pe": PE_LAT}.get(eng, X_LAT)

        tails = deque()
        gcnt = {}

        def enqueue_AB(t, base):
            items = build_AB(t)
            i_tail = len(items)
            for i, it in enumerate(items):
                if it[0] == "op" and it[1][0] == "dve" and "V16" in it[2].get("writes", ()):
                    i_tail = i
                    break
            acck = f"acc{t % 2}"
            barrier = float((t - 1) * 128)
            lw = {}
            lr = {}
            times = []
            t0 = max(float(base), floor_t[0])
            for it in items:
                eng = it[1][0] if it[0] == "op" else "dma"
                kk = it[2]
                reads, writes = kk.get("reads", ()), kk.get("writes", ())
                ready = t0
                if acck in writes:
                    ready = max(ready, barrier)
                for key in list(reads) + list(writes):
                    w = lw.get(key)
                    if w is not None:
                        ready = max(ready, w[1] + (lat_from(w[0]) if w[0] != eng else 0.0))
                for key in writes:
                    for (re_, rt_) in lr.get(key, ()):
                        ready = max(ready, rt_ + (lat_from(re_) if re_ != eng else 0.0))
                if eng == "dve":
                    while gcnt.get(int(ready), 0) >= BUDGET:
                        ready = float(int(ready) + 1)
                    gcnt[int(ready)] = gcnt.get(int(ready), 0) + 1
                times.append(ready)
                for key in reads:
                    lr.setdefault(key, []).append((eng, ready))
                for key in writes:
                    lw[key] = (eng, ready)
                    lr[key] = []
            order = sorted(range(i_tail), key=lambda i: (times[i], i))
            for i in order:
                pending.append((times[i], t, items[i]))
            order = sorted(range(i_tail, len(items)), key=lambda i: (times[i], i))
            for i in order:
                tails.append((times[i] + TAIL_DELAY, t, items[i]))
            floor_t[0] = max(times[:i_tail]) if i_tail else t0

        def emit_item(it):
            getattr(S, it[0])(*it[1], **it[2])

        def pop_ready(limit_tile=None):
            while True:
                progressed = False
                if pending and (pending[0][0] <= G[0] if limit_tile is None else pending[0][1] <= limit_tile):
                    it = pending[0][2]
                    if any(k_ in TOPK_KEYS for k_ in it[2].get("writes", ())):
                        while tails and tails[0][1] < pending[0][1]:
                            emit_item(tails.popleft()[2])
                    emit_item(pending.popleft()[2])
                    progressed = True
                if tails and (tails[0][0] <= G[0] if limit_tile is None else tails[0][1] <= limit_tile):
                    if not (pending and pending[0][1] <= tails[0][1]):
                        emit_item(tails.popleft()[2])
                        progressed = True
                if not progressed:
                    break

        def emit_CD(tt):
            p = tt % 2
            ACC, ACCk = acc[p], f"acc{p}"
            N2, N2k = n2[p], f"n2_{p}"
            IDX, IDXk = idx[p], f"idx{p}"
            GT, GTk = gates[p], f"gates{p}"
            r0 = tt * 128
            G[0] = tt * 128

            def advance():
                pop_ready()
                G[0] += 1

            for gi in range(128 // GRP):
                slots = []
                for jj in range(GRP):
                    j = gi * GRP + jj
                    b = rctr[0] % NR
                    rctr[0] += 1
                    slots.append((j, b))
                    rs = ring[:, b * 2048:(b + 1) * 2048]
                    S.dma("pool", lambda h, rs=rs, j=j: h.indirect_dma_start(
                        out=rs, out_offset=None, in_=tbl_d,
                        in_offset=bass.IndirectOffsetOnAxis(ap=IDX[:, j:j + 1], axis=0)),
                        f"g{b}", reads=([IDXk] + TBL_KEYS) if tt == 0 and j == 0 else [IDXk],
                        writes=[f"ring{b}", "stg0", "stg1"] if tt == 0 and j < NR else [f"ring{b}"])
                    S.op("dve", lambda h, rs=rs, j=j: h.scalar_tensor_tensor(
                        out=junk_d, in0=rs[:, 0:1024], scalar=1.0, in1=N2[:], op0=ALU.mult, op1=ALU.mult,
                        accum_out=hh[:, j:j + 1]),
                        reads=[f"ring{b}", N2k], writes=["jd", f"hh{gi}"])
                    advance()
                j0 = gi * GRP
                S.op("act", lambda h, j0=j0: h.activation(out=gl[:, j0:j0 + GRP], in_=hh[:, j0:j0 + GRP],
                                                          func=AF.Gelu_apprx_tanh), reads=[f"hh{gi}"], writes=["gl"])
                for (j, b) in slots:
                    di = dctr[0] % NDG
                    dctr[0] += 1
                    S.op("act", lambda h, j=j: h.activation(out=aw[:, j:j + 1], in_=gl[:, j:j + 1], func=AF.Copy,
                                                            scale=GT[:, j:j + 1]),
                         reads=["gl", GTk], writes=["aw"])
                    S.op("act", lambda h, j=j, di=di: h.activation(out=dg[:, di, :], in_=ident[:], func=AF.Copy,
                                                                   scale=aw[:, j:j + 1]),
                         reads=["ident", "aw"], writes=[f"dg{di}"])
                    for half in range(2):
                        S.op("pe", lambda h, j=j, b=b, di=di, half=half: h.matmul(
                            out=pacc[half][:], lhsT=dg[:, di, :],
                            rhs=ring[:, b * 2048 + 1024 + half * 512:b * 2048 + 1024 + (half + 1) * 512],
                            start=(j == 0), stop=(j == 127)),
                            reads=[f"dg{di}", f"ring{b}"], writes=[f"pacc{half}"])
            pop_ready(limit_tile=tt + 1)
            for half in range(2):
                S.op("dve", lambda h, half=half: h.tensor_tensor(
                    out=ACC[:, half * 512:(half + 1) * 512], in0=pacc[half][:], in1=ACC[:, half * 512:(half + 1) * 512],
                    op=ALU.add), reads=[f"pacc{half}", ACCk], writes=[ACCk])
            S.op("act", lambda h: h.activation(out=junk_a[:], in_=ACC[:], func=AF.Square, accum_out=sm3[:, 0:1]),
                 reads=[ACCk], writes=["junk_a", "sm3"])
            S.op("act", lambda h: h.activation(out=sm3[:, 1:2], in_=sm3[:, 0:1], func=AF.Ln, scale=1.0 / D, bias=EPS),
                 reads=["sm3"], writes=["sm3"])
            S.op("act", lambda h: h.activation(out=sm3[:, 1:2], in_=sm3[:, 1:2], func=AF.Exp, scale=-0.5),
                 reads=["sm3"], writes=["sm3"])
            S.op("dve", lambda h: h.scalar_tensor_tensor(out=N2[:], in0=ACC[:], scalar=sm3[:, 1:2], in1=cap("gFbc"),
                                                         op0=ALU.mult, op1=ALU.mult),
                 reads=[ACCk, "sm3", "cA"], writes=[N2k])
            S.dma("sp", lambda h: h.dma_start(out=y_d[r0:r0 + 128, :], in_=N2[:]), "sty", reads=[N2k],
                  writes=["ydram"])

        items = build_AB(0)
        run_items(items, 0, len(items))
        if NT > 1:
            enqueue_AB(1, 0)
            while pending:
                emit_item(pending.popleft()[2])
            floor_t[0] = 0.0
        for tt in range(NT):
            if tt + 2 < NT:
                enqueue_AB(tt + 2, (tt + 1) * 128 - EARLY)
            emit_CD(tt)

        sp = S.E["sp"]
        for key, d in S.dsem.items():
            if key == "sty" or key.startswith("tap_"):
                sp.prog.append(lambda h, d=d: h.wait_ge(d[0], d[1]))
        S.emit()
    return nc, tap_d


def _host_layout(inp):
    f = np.float32
    w_in = np.asarray(inp["w_in"], f)[0]
    wq = np.asarray(inp["peer_wq"], f)[0]
    wall = np.empty((NWALL, 128, 4096), f)

    def blockify(w):
        kc = w.shape[0] // 128
        return w.reshape(kc, 128, w.shape[1]).transpose(1, 0, 2).reshape(128, kc * w.shape[1])

    for b, c0 in enumerate(WIN_COL0):
        wall[b] = blockify(w_in[:, c0:c0 + 512])
    for b in range(4):
        wall[10 + b] = blockify(wq[:, b * 512:(b + 1) * 512])
    wall[14] = blockify(np.asarray(inp["w_branch_a"], f)[0])
    wall[15] = blockify(np.asarray(inp["w_branch_b"], f)[0])
    wo = np.asarray(inp["w_out"], f)[0]
    wall[16] = blockify(wo[0:512])
    wall[17] = blockify(wo[512:1024])

    cA = np.zeros((128, NCA), f)

    def put(name, arr):
        a, b = CA[name]
        cA[:arr.shape[0], a:b] = arr

    tri = np.zeros((128, 128), f)
    ci = np.zeros((128, 2), f)
    for cp in range(128):
        ci[cp, cp // 64] = -1.0 / 16.0
        for c in range(128):
            if cp // 64 == c // 64 and cp > c:
                tri[cp, c] = -1.0 / 16.0
    put("trirev", tri)
    put("chunkind", ci)
    put("iota16", np.broadcast_to(np.arange(16, dtype=f)[None], (128, 16)))
    put("g1T", np.asarray(inp["norm1_g"], f)[0].reshape(8, 128).T)
    put("g2T", np.asarray(inp["norm2_g"], f)[0].reshape(8, 128).T)
    put("goutT", np.asarray(inp["gla_norm_g"], f)[0].reshape(4, 128).T)
    put("sbT", np.asarray(inp["sgu_b"], f)[0].T)
    put("lng", np.broadcast_to(np.asarray(inp["sgu_ln_g"], f)[0][None], (128, 512)))
    put("lnb", np.broadcast_to(np.asarray(inp["sgu_ln_b"], f)[0][None], (128, 512)))
    put("g2bc", np.broadcast_to(np.asarray(inp["norm2_g"], f)[0][None], (128, 1024)))
    put("gFbc", np.broadcast_to(np.asarray(inp["final_g"], f)[None], (128, 1024)))
    put("wga", np.concatenate([np.asarray(inp["w_gate_up"], f)[0], np.asarray(inp["b_gate"], f)], axis=0))

    cB = np.zeros((128, NCB), f)

    def putb(name, arr):
        a, b = CB[name]
        cB[:, a:b] = arr

    putb("ident", np.eye(128, dtype=f))
    putb("k1T", np.asarray(inp["peer_k1"], f)[0].T)
    putb("k2T", np.asarray(inp["peer_k2"], f)[0].T)
    putb("swT", np.asarray(inp["sgu_w"], f)[0].transpose(2, 0, 1).reshape(128, 512))
    pos = np.arange(128) // 64
    putb("mask", (pos[None, :] >= pos[:, None]).astype(f))
    putb("walr", blockify(w_in[:, 2048:2064]))
    return wall, cA, cB


_NC_CACHE = {}


def kernel(**inputs):
    x = np.ascontiguousarray(np.asarray(inputs["x"], np.float32))
    wall, cA, cB = _host_layout(inputs)
    puv = np.concatenate([np.asarray(inputs["peer_u"], np.float32)[0],
                          np.asarray(inputs["peer_v"], np.float32)[0]], axis=1)
    if "nc" not in _NC_CACHE:
        _NC_CACHE["nc"] = build_nc(SEQ // 128)[0]
    nc = _NC_CACHE["nc"]
    in_maps = [{"x": x[b], "wall": wall, "cA": cA, "cB": cB, "peer_uv": puv} for b in range(NCORES)]
    res = run_bass_kernel_spmd(nc, in_maps, core_ids=list(range(NCORES)))
    return np.stack([np.asarray(r["y"], np.float32) for r in res.results], axis=0)
```
